# Optimizing a Trainium2 kernel written in Bass

```python
import jax, jax.numpy as jnp
from jax import lax
import numpy as np

D_MODEL = 1024
BATCH = 16
SEQ = 2048
DEPTH = 1
DEC_BATCH = 16
DEC_SEQ = 64
PAST_LEN = 1024

CHUNK = 64
N_MEM = 256
EPS = 1e-6
ML_HEADS = 4
ML_DQK = 128
ML_DV = D_MODEL // ML_HEADS
FORGET_BIAS = 3.0
SGU_CHUNK = 128
SGU_GROUPS = 4
SGU_DIM = D_MODEL
XA_HEADS = 4
XA_DH = D_MODEL // XA_HEADS
N_BRANCH = 3
PEER_HEADS = 8
PEER_NKEYS = 128
PEER_DQ = 256
PEER_TOPK = 16
PEER_NEXP = PEER_NKEYS * PEER_NKEYS
PEER_BLOCK = 256
D_IN = 2 * ML_HEADS * ML_DQK + 2 * ML_HEADS * ML_DV + 2 * ML_HEADS + 2 * SGU_DIM + XA_HEADS * XA_DH + N_BRANCH * D_MODEL
F_START = 2 * ML_HEADS * ML_DQK + ML_HEADS * ML_DV + ML_HEADS

kernel_name = "mlstm_sgu_memxattn_peer_stream_step"


def _split_points():
    sizes = (ML_HEADS * ML_DQK, ML_HEADS * ML_DQK, ML_HEADS * ML_DV, ML_HEADS, ML_HEADS,
             ML_HEADS * ML_DV, SGU_DIM, SGU_DIM, XA_HEADS * XA_DH)
    pts, acc = [], 0
    for s in sizes:
        acc += s
        pts.append(acc)
    return pts


def _rmsnorm(x, g):
    xf = x.astype(jnp.float32)
    y = xf * lax.rsqrt(jnp.mean(xf * xf, axis=-1, keepdims=True) + EPS) * g.astype(jnp.float32)
    return y.astype(x.dtype)


def _mlstm(q, k, v, ig, lf, C0, n0, m0):
    B, S, H, _ = q.shape
    L = min(S, CHUNK)
    nc = S // L
    tril = jnp.tril(jnp.ones((L, L), dtype=bool))

    def to_chunks(a):
        return jnp.moveaxis(a.reshape((B, nc, L) + a.shape[2:]), 1, 0)

    def step(carry, inp):
        C, n, m = carry
        qc, kc, vc, igc, lfc = inp
        b = jnp.cumsum(lfc, axis=1)
        logD = b[:, :, None, :] - b[:, None, :, :] + igc[:, None, :, :]
        logD = jnp.where(tril[None, :, :, None], logD, -jnp.inf)
        log_prev = b + m[:, None, :]
        m_t = jnp.maximum(log_prev, jnp.max(logD, axis=2))
        A = jnp.einsum('bthd,bshd->btsh', qc, kc) * jnp.exp(logD - m_t[:, :, None, :])
        wp = jnp.exp(log_prev - m_t)
        num = jnp.einsum('btsh,bshv->bthv', A, vc) + wp[..., None] * jnp.einsum('bthd,bhdv->bthv', qc, C)
        den = jnp.sum(A, axis=2) + wp * jnp.einsum('bthd,bhd->bth', qc, n)
        h = num / jnp.maximum(jnp.abs(den), jnp.exp(-m_t))[..., None]
        bL = b[:, -1, :]
        log_in = bL[:, None, :] - b + igc
        m_new = jnp.maximum(bL + m, jnp.max(log_in, axis=1))
        wi = jnp.exp(log_in - m_new[:, None, :])
        wc = jnp.exp(bL + m - m_new)
        C_new = wc[..., None, None] * C + jnp.einsum('bsh,bshd,bshv->bhdv', wi, kc, vc)
        n_new = wc[..., None] * n + jnp.einsum('bsh,bshd->bhd', wi, kc)
        return (C_new, n_new, m_new), h

    xs = (to_chunks(q), to_chunks(k), to_chunks(v), to_chunks(ig), to_chunks(lf))
    (C1, n1, m1), h = lax.scan(step, (C0, n0, m0), xs)
    h = jnp.moveaxis(h, 0, 1).reshape(B, S, H, ML_DV)
    return h, C1, n1, m1


def _mem_kv(mem, g_mem, w_mk, w_mv):
    B = mem.shape[0]
    mn = _rmsnorm(mem, g_mem)
    mk = (mn @ w_mk).reshape(B, N_MEM, XA_HEADS, XA_DH)
    mv = (mn @ w_mv).reshape(B, N_MEM, XA_HEADS, XA_DH)
    return mk, mv


def _peer(xn, w_pq, k_sub1, k_sub2, u_exp, v_exp):
    B, S, D = xn.shape
    T = B * S
    nb = -(-T // PEER_BLOCK)
    xf = jnp.pad(xn.reshape(T, D), ((0, nb * PEER_BLOCK - T), (0, 0))).reshape(nb, PEER_BLOCK, D)
    half = PEER_DQ // 2

    def block(xb):
        q = (xb @ w_pq).reshape(PEER_BLOCK, PEER_HEADS, PEER_DQ)
        s1 = jnp.einsum('thd,kd->thk', q[..., :half], k_sub1).astype(jnp.float32)
        s2 = jnp.einsum('thd,kd->thk', q[..., half:], k_sub2).astype(jnp.float32)
        v1, i1 = lax.top_k(s1, PEER_TOPK)
        v2, i2 = lax.top_k(s2, PEER_TOPK)
        cand = (v1[..., :, None] + v2[..., None, :]).reshape(PEER_BLOCK, PEER_HEADS, PEER_TOPK * PEER_TOPK)
        cidx = (i1[..., :, None] * PEER_NKEYS + i2[..., None, :]).reshape(PEER_BLOCK, PEER_HEADS, PEER_TOPK * PEER_TOPK)
        sc, pos = lax.top_k(cand, PEER_TOPK)
        e = jnp.take_along_axis(cidx, pos, axis=-1).reshape(PEER_BLOCK, PEER_HEADS * PEER_TOPK)
        g = jax.nn.softmax(sc, axis=-1).reshape(PEER_BLOCK, PEER_HEADS * PEER_TOPK)
        a = jax.nn.gelu(jnp.einsum('tkd,td->tk', u_exp[e], xb).astype(jnp.float32))
        return jnp.einsum('tk,tkd->td', (g * a).astype(xb.dtype), v_exp[e])

    out = lax.map(block, xf).reshape(nb * PEER_BLOCK, D)[:T]
    return out.reshape(B, S, D)


def _layer(x, mem_k, mem_v, C0, n0, m0, g_mix, w_in, b_in, g_mlh, g_sgu, w_s, b_s, w_out,
           g_ffn, w_pq, k_sub1, k_sub2, u_exp, v_exp):
    B, S, D = x.shape
    xn = _rmsnorm(x, g_mix)
    p = xn @ w_in + b_in
    q, k, v, ig, fg, og, su, sv, xq, gates = jnp.split(p, _split_points(), axis=-1)

    f32 = jnp.float32
    qm = q.reshape(B, S, ML_HEADS, ML_DQK).astype(f32)
    km = k.reshape(B, S, ML_HEADS, ML_DQK).astype(f32) * (ML_DQK ** -0.5)
    vm = v.reshape(B, S, ML_HEADS, ML_DV).astype(f32)
    lf = jax.nn.log_sigmoid(fg.astype(f32))
    h, C1, n1, m1 = _mlstm(qm, km, vm, ig.astype(f32), lf, C0, n0, m0)
    h = h * lax.rsqrt(jnp.mean(h * h, axis=-1, keepdims=True) + EPS)
    h_a = (jax.nn.sigmoid(og.astype(f32)) * h.reshape(B, S, D) * g_mlh.astype(f32)).astype(x.dtype)

    u = jax.nn.gelu(su)
    vn = _rmsnorm(jax.nn.gelu(sv), g_sgu)
    L = min(S, SGU_CHUNK)
    nc = S // L
    vg = vn.reshape(B, nc, L, SGU_GROUPS, SGU_DIM // SGU_GROUPS)
    ws = w_s[:, :L, :L] * jnp.tril(jnp.ones((L, L), dtype=w_s.dtype))
    mix = jnp.einsum('gts,bnsgc->bntgc', ws, vg) + b_s[:, :L].T[None, None, :, :, None]
    h_b = u * mix.reshape(B, S, SGU_DIM)

    xq = xq.reshape(B, S, XA_HEADS, XA_DH)
    sc = jnp.einsum('bshd,bmhd->bhsm', xq, mem_k).astype(f32) * (XA_DH ** -0.5)
    att = jax.nn.softmax(sc, axis=-1).astype(x.dtype)
    h_c = jnp.einsum('bhsm,bmhd->bshd', att, mem_v).reshape(B, S, D)

    g = jax.nn.sigmoid(gates.astype(f32)).reshape(B, S, N_BRANCH, D)
    merged = (g[:, :, 0] * h_a + g[:, :, 1] * h_b + g[:, :, 2] * h_c).astype(x.dtype)
    x = x + merged @ w_out

    x = x + _peer(_rmsnorm(x, g_ffn), w_pq, k_sub1, k_sub2, u_exp, v_exp)
    return x, C1, n1, m1, vn


def setup_inputs(seed: int = 0) -> dict:
    key = jax.random.key(seed)
    ks = jax.random.split(key, 28)
    f32 = jnp.float32

    def nrm(k, shape, scale):
        return scale * jax.random.normal(k, shape, f32)

    def gain(k, shape):
        return 1.0 + 0.02 * jax.random.normal(k, shape, f32)

    D = D_MODEL
    b_in = nrm(ks[11], (DEPTH, D_IN), 0.02).at[:, F_START:F_START + ML_HEADS].add(FORGET_BIAS)
    return {
        "x_prompt": nrm(ks[0], (BATCH, SEQ, D), 1.0),
        "x_sample": nrm(ks[1], (DEC_BATCH, DEC_SEQ, D), 1.0),
        "mem_prompt": nrm(ks[2], (BATCH, N_MEM, D), 1.0),
        "cache_mem_k": nrm(ks[3], (DEPTH, DEC_BATCH, N_MEM, XA_HEADS, XA_DH), 1.0),
        "cache_mem_v": nrm(ks[4], (DEPTH, DEC_BATCH, N_MEM, XA_HEADS, XA_DH), 1.0),
        "state_mlstm_C": nrm(ks[5], (DEPTH, DEC_BATCH, ML_HEADS, ML_DQK, ML_DV), 0.05),
        "state_mlstm_n": nrm(ks[6], (DEPTH, DEC_BATCH, ML_HEADS, ML_DQK), 0.1),
        "state_mlstm_m": nrm(ks[7], (DEPTH, DEC_BATCH, ML_HEADS), 1.0),
        "g_mix": gain(ks[8], (DEPTH, D)),
        "w_in": nrm(ks[9], (DEPTH, D, D_IN), D ** -0.5),
        "b_in": b_in,
        "g_mlh": gain(ks[10], (DEPTH, D)),
        "g_sgu": gain(ks[12], (DEPTH, SGU_DIM)),
        "w_s": nrm(ks[13], (DEPTH, SGU_GROUPS, SGU_CHUNK, SGU_CHUNK), 0.5 * SGU_CHUNK ** -0.5),
        "b_s": gain(ks[14], (DEPTH, SGU_GROUPS, SGU_CHUNK)),
        "g_mem": gain(ks[15], (DEPTH, D)),
        "w_mk": nrm(ks[16], (DEPTH, D, XA_HEADS * XA_DH), D ** -0.5),
        "w_mv": nrm(ks[17], (DEPTH, D, XA_HEADS * XA_DH), D ** -0.5),
        "w_out": nrm(ks[18], (DEPTH, D, D), 0.5 * D ** -0.5),
        "g_ffn": gain(ks[19], (DEPTH, D)),
        "w_pq": nrm(ks[20], (DEPTH, D, PEER_HEADS * PEER_DQ), D ** -0.5),
        "k_sub1": nrm(ks[21], (DEPTH, PEER_NKEYS, PEER_DQ // 2), (PEER_DQ // 2) ** -0.5),
        "k_sub2": nrm(ks[22], (DEPTH, PEER_NKEYS, PEER_DQ // 2), (PEER_DQ // 2) ** -0.5),
        "u_exp": nrm(ks[23], (DEPTH, PEER_NEXP, D), D ** -0.5),
        "v_exp": nrm(ks[24], (DEPTH, PEER_NEXP, D), PEER_HEADS ** -0.5),
        "g_final": gain(ks[25], (D,)),
    }


def reference(x_prompt, x_sample, mem_prompt, cache_mem_k, cache_mem_v, state_mlstm_C, state_mlstm_n,
              state_mlstm_m, g_mix, w_in, b_in, g_mlh, g_sgu, w_s, b_s, g_mem, w_mk, w_mv, w_out, g_ffn,
              w_pq, k_sub1, k_sub2, u_exp, v_exp, g_final):
    f32 = jnp.float32
    Bp = x_prompt.shape[0]
    xp, xs = x_prompt, x_sample
    Cp, Np, Mp, MKp, MVp, Cs, Ns, Ms, Vs = [], [], [], [], [], [], [], [], []
    for l in range(DEPTH):
        w = (g_mix[l], w_in[l], b_in[l], g_mlh[l], g_sgu[l], w_s[l], b_s[l], w_out[l], g_ffn[l],
             w_pq[l], k_sub1[l], k_sub2[l], u_exp[l], v_exp[l])
        mk, mv = _mem_kv(mem_prompt, g_mem[l], w_mk[l], w_mv[l])
        C0 = jnp.zeros((Bp, ML_HEADS, ML_DQK, ML_DV), f32)
        n0 = jnp.zeros((Bp, ML_HEADS, ML_DQK), f32)
        m0 = jnp.zeros((Bp, ML_HEADS), f32)
        xp, c1, n1, m1, _ = _layer(xp, mk, mv, C0, n0, m0, *w)
        Cp.append(c1); Np.append(n1); Mp.append(m1); MKp.append(mk); MVp.append(mv)
        xs, c2, n2, m2, vn = _layer(xs, cache_mem_k[l], cache_mem_v[l], state_mlstm_C[l].astype(f32),
                                    state_mlstm_n[l].astype(f32), state_mlstm_m[l].astype(f32), *w)
        Cs.append(c2); Ns.append(n2); Ms.append(m2); Vs.append(vn)
    y_prompt = _rmsnorm(xp, g_final)
    y_sample = _rmsnorm(xs, g_final)
    new_mlstm_C_prompt = jnp.stack(Cp)
    new_mlstm_n_prompt = jnp.stack(Np)
    new_mlstm_m_prompt = jnp.stack(Mp)
    new_mem_k_prompt = jnp.stack(MKp)
    new_mem_v_prompt = jnp.stack(MVp)
    new_mlstm_C_sample = jnp.stack(Cs)
    new_mlstm_n_sample = jnp.stack(Ns)
    new_mlstm_m_sample = jnp.stack(Ms)
    new_sgu_v_sample = jnp.stack(Vs)
    return (y_prompt, y_sample, new_mlstm_C_prompt, new_mlstm_n_prompt, new_mlstm_m_prompt,
            new_mem_k_prompt, new_mem_v_prompt, new_mlstm_C_sample, new_mlstm_n_sample,
            new_mlstm_m_sample, new_sgu_v_sample)
```

```python
import math
import numpy as np
from contextlib import ExitStack
import concourse.bass as bass
import concourse.mybir as mybir
from concourse.bass_utils import run_bass_kernel_spmd

F32 = mybir.dt.float32
BF16 = mybir.dt.bfloat16
U32 = mybir.dt.uint32
I32 = mybir.dt.int32
AF = mybir.ActivationFunctionType
ALU = mybir.AluOpType
AX = mybir.AxisListType
EPS = 1e-6
import os as _os
PIPE_B = _os.environ.get("PIPE_B", "1") == "1"
PIPE_C = _os.environ.get("PIPE_C", "1") == "1"


class Unit:
    __slots__ = ("name", "lw", "rd")

    def __init__(self, name):
        self.name = name
        self.lw = None
        self.rd = []


class T:
    def __init__(self, ap, name):
        self.ap = ap
        self.u = Unit(name)

    def __getitem__(self, k):
        return self.ap[k]


class Op:
    __slots__ = ("eng", "emit", "deps", "idx", "dma", "sem", "sigval", "needed", "dwait")


class Prog:
    ENGS = ("sp", "act", "dve", "pool", "pe")

    def __init__(self, nc, es, dma_keys):
        self.nc = nc
        self.es = es
        self.ops = []
        self.by_eng = {e: [] for e in self.ENGS}
        self.eng_sem = {e: es.enter_context(nc.semaphore("s_" + e)) for e in self.ENGS}
        self.dsem = {k: es.enter_context(nc.semaphore("d_" + k)) for k in dma_keys}
        self.cnt = {e: 0 for e in self.ENGS}
        self.dcnt = {k: 0 for k in dma_keys}
        self.waited = {e: {} for e in self.ENGS}
        self.flushed = 0

    def sb(self, es, name, shape, dtype):
        h = es.enter_context(self.nc.sbuf_tensor(name, list(shape), dtype))
        return T(h[tuple(slice(None) for _ in shape)], name)

    def ps(self, es, name, shape, dtype):
        h = es.enter_context(self.nc.psum_tensor(name, list(shape), dtype))
        return T(h[tuple(slice(None) for _ in shape)], name)

    def add(self, eng, emit, reads=(), writes=(), dma=None):
        op = Op()
        op.eng = eng
        op.emit = emit
        op.idx = len(self.ops)
        op.dma = dma
        op.needed = dma is not None
        op.sem = None
        op.sigval = None
        deps = set()
        for t in reads:
            u = t.u
            if u.lw is not None:
                deps.add(u.lw)
        for t in writes:
            u = t.u
            if u.lw is not None:
                deps.add(u.lw)
            deps.update(u.rd)
        for t in reads:
            t.u.rd.append(op.idx)
        for t in writes:
            t.u.lw = op.idx
            t.u.rd = []
        deps.discard(op.idx)
        op.deps = deps
        op.dwait = {}
        for d in deps:
            dk = self.ops[d].dma
            if dk is not None:
                op.dwait[dk] = self.dcnt[dk]
        if dma is not None:
            self.dcnt[dma] += 16
            op.sem = self.dsem[dma]
            op.sigval = self.dcnt[dma]
        self.ops.append(op)
        self.by_eng[eng].append(op)
        return op

    def dma(self, out, in_, reads, writes, key, eng="sp", **kw):
        return self.add(eng, lambda e: e.dma_start(out=out, in_=in_, **kw), reads, writes, dma=key)

    def tt(self, eng, out, in0, in1, op, reads, writes):
        return self.add(eng, lambda e: e.tensor_tensor(out=out, in0=in0, in1=in1, op=op), reads, writes)

    def ts(self, eng, out, in0, s1, s2, op0, op1, reads, writes, accum=None):
        if op1 is None:
            return self.add(eng, lambda e: e.tensor_scalar(out=out, in0=in0, scalar1=s1, scalar2=None, op0=op0),
                            reads, writes)
        if accum is not None:
            return self.add(eng, lambda e: e.tensor_scalar(out=out, in0=in0, scalar1=s1, scalar2=s2, op0=op0,
                                                          op1=op1, accum_out=accum), reads, writes)
        return self.add(eng, lambda e: e.tensor_scalar(out=out, in0=in0, scalar1=s1, scalar2=s2, op0=op0, op1=op1),
                        reads, writes)

    def stt(self, eng, out, in0, scalar, in1, op0, op1, reads, writes):
        return self.add(eng, lambda e: e.scalar_tensor_tensor(out=out, in0=in0, scalar=scalar, in1=in1, op0=op0,
                                                             op1=op1), reads, writes)

    def act(self, out, in_, func, reads, writes, bias=0.0, scale=1.0, accum=None):
        if accum is not None:
            return self.add("act", lambda e: e.activation(out=out, in_=in_, func=func, bias=bias, scale=scale,
                                                          accum_out=accum), reads, writes)
        return self.add("act", lambda e: e.activation(out=out, in_=in_, func=func, bias=bias, scale=scale),
                        reads, writes)

    def copy(self, eng, out, in_, reads, writes):
        if eng == "act":
            return self.add("act", lambda e: e.copy(out=out, in_=in_), reads, writes)
        return self.add(eng, lambda e: e.tensor_copy(out=out, in_=in_), reads, writes)

    def memset(self, eng, ap, val, writes):
        return self.add(eng, lambda e: e.memset(ap, val), (), writes)

    def recip(self, out, in_, reads, writes):
        return self.add("dve", lambda e: e.reciprocal(out=out, in_=in_), reads, writes)

    def mm(self, lst, reads, writes):
        def emit(e):
            ins = None
            for (o, l, r, st, sp) in lst:
                ins = e.matmul(o, lhsT=l, rhs=r, start=st, stop=sp)
            return ins
        return self.add("pe", emit, reads, writes)

    def tr(self, lst, reads, writes):
        def emit(e):
            ins = None
            for (o, i, idn) in lst:
                ins = e.transpose(out=o, in_=i, identity=idn)
            return ins
        return self.add("pe", emit, reads, writes)

    def barrier(self):
        last = set()
        seen_e = set()
        seen_k = set()
        for op in reversed(self.ops[self.flushed:]):
            if op.emit is None:
                continue
            if op.dma is not None:
                if op.dma not in seen_k:
                    seen_k.add(op.dma)
                    last.add(op.idx)
            elif op.eng not in seen_e:
                seen_e.add(op.eng)
                last.add(op.idx)
        for e in self.ENGS:
            op = self.add(e, None)
            op.deps = set(last)
            op.dwait = dict(self.dcnt)

    def phase_end(self, final=False):
        self.barrier()
        self.flush(final)

    def flush(self, final=False):
        nc = self.nc
        ops = self.ops
        start = self.flushed
        new = ops[start:]

        def skip(op, dop):
            return dop.eng == "pe" and op.eng == "pe" and dop.dma is None and op.dma is None

        for op in new:
            for d in op.deps:
                if d < start:
                    continue
                dop = ops[d]
                if skip(op, dop):
                    continue
                dop.needed = True
        for op in new:
            if op.emit is None:
                continue
            if op.dma is not None:
                pass
            elif op.needed:
                self.cnt[op.eng] += 1
                op.sem = self.eng_sem[op.eng]
                op.sigval = self.cnt[op.eng]
        self.flushed = len(ops)

        def run(engname, e):
            waited = self.waited[engname]
            for op in self.by_eng[engname]:
                want = {}
                for d in op.deps:
                    if d < start:
                        continue
                    dop = ops[d]
                    if skip(op, dop) or dop.sigval is None:
                        continue
                    k = id(dop.sem)
                    v = op.dwait[dop.dma] if dop.dma is not None else dop.sigval
                    if k not in want or want[k][1] < v:
                        want[k] = (dop.sem, v)
                for k, (s, v) in want.items():
                    if waited.get(k, 0) >= v:
                        continue
                    e.wait_ge(s, v)
                    waited[k] = v
                if op.emit is None:
                    continue
                ins = op.emit(e)
                if op.sigval is not None:
                    ins.then_inc(op.sem, 16 if op.dma is not None else 1)
            if final and engname == "sp":
                for k, s in self.dsem.items():
                    if self.dcnt[k] > 0:
                        e.wait_ge(s, self.dcnt[k])

        with nc.Block() as block:
            @block.sync
            def _(e):
                run("sp", e)

            @block.scalar
            def _(e):
                run("act", e)

            @block.vector
            def _(e):
                run("dve", e)

            @block.gpsimd
            def _(e):
                run("pool", e)

            @block.tensor
            def _(e):
                run("pe", e)
        self.by_eng = {e: [] for e in self.ENGS}


C_IDENT, C_TRIU, C_TRIUS, C_ONES, C_R0, C_R1, C_IOTA, C_HM0ROW, C_HM1ROW, C_MISC = range(10)
NCONST = 10


def make_consts():
    c = np.zeros((128, NCONST, 128), np.float32)
    s = np.arange(128)[:, None]
    t = np.arange(128)[None, :]
    c[:, C_IDENT] = (s == t)
    c[:, C_TRIU] = (s <= t)
    c[:, C_TRIUS] = (s <= t) & ((s // 64) == (t // 64))
    c[:, C_ONES] = 1.0
    c[:, C_R0] = (s < 64) * np.ones_like(t)
    c[:, C_R1] = (s >= 64) * np.ones_like(t)
    c[:, C_IOTA] = t * np.ones_like(s)
    c[:, C_HM0ROW] = (t < 64) * np.ones_like(s)
    c[:, C_HM1ROW] = (t >= 64) * np.ones_like(s)
    c[:, C_MISC, 0] = (np.arange(128) < 64)
    c[:, C_MISC, 1] = (np.arange(128) >= 64)
    return c.reshape(128, NCONST * 128)


Q0, K0, V0, IG0, FG0, OG0, SU0, SV0, XQ0, G00, G10, G20, DIN = 0, 512, 1024, 2048, 2052, 2056, 3080, 4104, 5128, 6152, 7176, 8200, 9224

DMA_KEYS = ["c", "ws0", "ws1", "x0", "x1", "st0", "st1", "o0", "o1", "sc0", "sc1", "ld0", "ld1", "ld2", "ld3",
            "misc0", "misc1", "oC", "pst0", "pst1", "p3"]


def build(NTP=16, peer=True):
    NT = 2 * NTP + 1
    NTOK = NT * 128
    nc = bass.Bass("TRN2", target_bir_lowering=False)

    def din(name, shape, dt=F32):
        return nc.dram_tensor(name, list(shape), dt, kind="ExternalInput").ap()

    def dout(name, shape, dt=F32):
        return nc.dram_tensor(name, list(shape), dt, kind="ExternalOutput").ap()

    def dscr(name, shape, dt=F32):
        return nc.dram_tensor(name, list(shape), dt, kind="Internal").ap()

    xall = din("xall", [NTOK, 1024])
    mem = din("mem", [512, 1024])
    cmk = din("cmk", [2, 256, 1024])
    cmv = din("cmv", [2, 256, 1024])
    sC = din("sC", [2, 4, 128, 256])
    sn = din("sn", [2, 4, 128])
    sm = din("sm", [2, 4])
    g_mix = din("g_mix", [1024])
    w_in = din("w_in", [1024, DIN])
    b_in = din("b_in", [DIN])
    g_mlh = din("g_mlh", [1024])
    g_sgu = din("g_sgu", [1024])
    w_s = din("w_s", [4, 128, 128])
    b_s = din("b_s", [4, 128])
    g_mem = din("g_mem", [1024])
    w_mk = din("w_mk", [1024, 1024])
    w_mv = din("w_mv", [1024, 1024])
    w_out = din("w_out", [1024, 1024])
    g_ffn = din("g_ffn", [1024])
    w_pq = din("w_pq", [1024, 2048])
    k_sub1 = din("k_sub1", [128, 128])
    k_sub2 = din("k_sub2", [128, 128])
    u_expT = din("u_expT", [1024, 16384])
    v_exp = din("v_exp", [16384, 1024])
    g_final = din("g_final", [1024])
    consts_d = din("consts", [128, NCONST * 128])

    y_o = dout("y", [NTOK, 1024])
    Cp_o = dout("Cp", [2, 4, 128, 256])
    np_o = dout("np", [2, 4, 128])
    mp_o = dout("mp", [2, 4])
    mk_o = dout("mk", [2, 256, 1024])
    mv_o = dout("mv", [2, 256, 1024])
    Cs_o = dout("Cs", [2, 4, 128, 256])
    ns_o = dout("ns", [2, 4, 128])
    ms_o = dout("ms", [2, 4])
    sguv_o = dout("sguv", [128, 1024])

    xnT_d = T(dscr("xnT_d", [NT, 128, 1024], BF16), "xnT_d")
    ma_d = T(dscr("ma_d", [NTOK, 1024]), "ma_d")
    x2_d = T(dscr("x2_d", [NTOK, 1024]), "x2_d")
    u_bf = T(dscr("u_bf", [32, 128, 8, 512], BF16), "u_bf")
    v_bf = T(dscr("v_bf", [32, 128, 4, 1024], BF16), "v_bf")
    mk_u = T(mk_o, "mk_o")
    mv_u = T(mv_o, "mv_o")

    with ExitStack() as es0:
        P = Prog(nc, es0, DMA_KEYS)
        cst = P.sb(es0, "cst", [128, NCONST, 128], F32)
        identb = P.sb(es0, "identb", [128, 128], BF16)
        P.dma(cst[:].rearrange("p a b -> p (a b)"), consts_d, [], [cst], "misc1")
        P.copy("dve", identb[:], cst[:, C_IDENT, :], [cst], [identb])
        identf = cst[:, C_IDENT, :]

        def rmsnorm_T(es_unused, x_ap, x_t, junk, ss, rstd, xn, pT, xnT, eng_mul="dve"):
            P.act(junk[:], x_ap, AF.Square, [x_t], [junk, ss], accum=ss[:])
            P.act(rstd[:], ss[:], AF.Sqrt, [ss], [rstd], bias=EPS, scale=1.0 / 1024)
            P.recip(rstd[:], rstd[:], [rstd], [rstd])
            P.ts(eng_mul, xn[:], x_ap, rstd[:, 0:1], None, ALU.mult, None, [x_t, rstd], [xn])
            P.tr([(pT[:, c, :], xn[:, c * 128:(c + 1) * 128], identb[:]) for c in range(8)], [xn, identb], [pT])
            P.copy("act", xnT[:], pT[:], [pT], [xnT])

        def load_w(es, wt, w_dram, col_ranges, gvec, stg, nrow_chunks=8):
            i = 0
            for c in range(nrow_chunks):
                o = 0
                for (a, b) in col_ranges:
                    p = a
                    while p < b:
                        n = min(1024, b - p)
                        s = stg[i % 2]
                        P.dma(s[:, 0:n], w_dram[c * 128:(c + 1) * 128, p:p + n], [], [s], f"ws{i%2}")
                        if i % 2:
                            P.act(wt[:, c, o:o + n], s[:, 0:n], AF.Copy, [s, gvec], [wt], scale=gvec[:, c:c + 1])
                        else:
                            P.ts("dve", wt[:, c, o:o + n], s[:, 0:n], gvec[:, c:c + 1], None, ALU.mult, None,
                                 [s, gvec], [wt])
                        o += n
                        p += n
                        i += 1


        class P0:
            def __init__(self):
                self.next_piece = 0
                self.pending = []
                self.cs = self.cb = self.lk = None
                self.sk = ["sc0", "sc1", "p3"]

            def bind(self, cs, cb, lk):
                assert not self.pending
                self.cs, self.cb, self.lk = cs, cb, lk

            def load_some(self, n=3):
                for i in range(n):
                    if self.next_piece >= 128:
                        return
                    pc = self.next_piece
                    self.next_piece += 1
                    c_ = self.cs[i]
                    if pc < 64:
                        r0, c0 = (pc // 8) * 128, (pc % 8) * 2048
                        P.dma(c_[:], u_expT[r0:r0 + 128, c0:c0 + 2048], [], [c_], self.lk[i])
                    else:
                        q = pc - 64
                        src = v_exp[q * 256:(q + 1) * 256, :].rearrange("(a p) d -> p a d", a=2)
                        P.dma(c_[:].rearrange("p (a d) -> p a d", a=2), src, [], [c_], self.lk[i])
                    self.pending.append((pc, i))

            def cast_some(self):
                for (pc, i) in self.pending:
                    c_, b_ = self.cs[i], self.cb[i]
                    P.copy("act", b_[:], c_[:], [c_], [b_])
                    if pc < 64:
                        b0 = (pc % 8) * 4
                        P.dma(u_bf[b0:b0 + 4, :, pc // 8, :].rearrange("b p e -> p b e"),
                              b_[:].rearrange("p (b e) -> p b e", b=4), [b_], [u_bf], self.sk[i], eng="pool")
                    else:
                        q = pc - 64
                        dstv = v_bf[q // 2, :, (q % 2) * 2:(q % 2) * 2 + 2, :]
                        P.dma(dstv, b_[:].rearrange("p (a d) -> p a d", a=2), [b_], [v_bf], self.sk[i], eng="pool")
                self.pending = []

            def step(self):
                self.cast_some()
                self.load_some()

        p0 = P0() if peer else None

        with ExitStack() as es:
            gm = P.sb(es, "gm", [128, 8], F32)
            stg = [P.sb(es, f"stg{i}", [128, 1024], F32) for i in range(2)]
            wk = P.sb(es, "wk", [128, 8, 1024], BF16)
            wv = P.sb(es, "wv", [128, 8, 1024], BF16)
            xt = [P.sb(es, f"xt{i}", [128, 1024], F32) for i in range(2)]
            junk = P.sb(es, "junk", [128, 1024], F32)
            ss = P.sb(es, "ss", [128, 1], F32)
            rstd = P.sb(es, "rstd", [128, 1], F32)
            xn = P.sb(es, "xn", [128, 1024], BF16)
            xnT = P.sb(es, "xnT", [128, 8, 128], BF16)
            ot = [P.sb(es, f"ot{i}", [128, 1024], F32) for i in range(2)]
            pT = P.ps(es, "pT", [128, 8, 128], BF16)
            pm = [P.ps(es, f"pm{i}", [128, 512], F32) for i in range(2)]
            P.dma(gm[:], g_mem.rearrange("(c p) -> p c", p=128), [], [gm], "c", allow_slow_non_contiguous=True)
            load_w(es, wk, w_mk, [(0, 1024)], gm, stg)
            load_w(es, wv, w_mv, [(0, 1024)], gm, stg)
            oi = 0
            for t in range(4):
                x = xt[t % 2]
                P.dma(x[:], mem[t * 128:(t + 1) * 128, :], [], [x], f"x{t%2}")
                rmsnorm_T(es, x[:], x, junk, ss, rstd, xn, pT, xnT)
                for (wt, o_dram, o_u) in ((wk, mk_o, mk_u), (wv, mv_o, mv_u)):
                    o = ot[oi % 2]
                    for h in range(2):
                        P.mm([(pm[h][:], xnT[:, c, :], wt[:, c, h * 512:(h + 1) * 512], c == 0, c == 7)
                              for c in range(8)], [xnT, wt], [pm[h]])
                        P.copy("dve" if h == 0 else "act", o[:, h * 512:(h + 1) * 512], pm[h][:], [pm[h]], [o])
                    P.dma(o_dram[t // 2, (t % 2) * 128:(t % 2 + 1) * 128, :], o[:], [o], [o_u], f"o{oi%2}", eng="pool")
                    oi += 1
            P.phase_end()

        with ExitStack() as es:
            NA = 4104
            gm = P.sb(es, "gmA", [128, 8], F32)
            stg = [P.sb(es, f"stgA{i}", [128, 1024], F32) for i in range(2)]
            wA = P.sb(es, "wA", [128, 8, NA], BF16)
            oQ, oK, oV, oIG, oOG, oG0 = 0, 512, 1024, 2048, 2056, 3080
            bq = P.sb(es, "bq", [128, 8], F32)
            btok = P.sb(es, "btokA", [128, NA], F32)
            gmlh = P.sb(es, "gmlh", [128, 1024], F32)
            xt = [P.sb(es, f"xA{i}", [128, 1024], F32) for i in range(2)]
            junk = P.sb(es, "junkA", [128, 1024], F32)
            ss = P.sb(es, "ssA", [128, 1], F32)
            rstd = P.sb(es, "rstdA", [128, 1], F32)
            xn = P.sb(es, "xnA", [128, 1024], BF16)
            xnT = [P.sb(es, f"xnTA{i}", [128, 8, 128], BF16) for i in range(2)]
            qT = P.sb(es, "qT", [128, 4, 128], BF16)
            kT = P.sb(es, "kT", [128, 4, 128], BF16)
            qTm = [P.sb(es, f"qTm{i}", [128, 4, 128], BF16) for i in range(2)]
            ktok = P.sb(es, "ktok", [128, 512], F32)
            kts = [P.sb(es, f"kts{i}", [128, 512], BF16) for i in range(2)]
            vaug = P.sb(es, "vaug", [128, 4, 257], BF16)
            gsb = P.sb(es, "gsb", [128, 8], F32)
            gw = P.sb(es, "gw", [128, 40], F32)
            gateA = P.sb(es, "gateA", [128, 1024], F32)
            g0s = P.sb(es, "g0s", [128, 1024], F32)
            mat = [P.sb(es, f"mat{i}", [128, 1024], F32) for i in range(2)]
            Cf = [P.sb(es, f"Cf{i}", [128, 4, 257], F32) for i in range(2)]
            Cb = [P.sb(es, f"Cb{i}", [128, 4, 257], BF16) for i in range(2)]
            Cout = P.sb(es, "Cout", [128, 4, 257], F32)
            mst = [P.sb(es, f"mst{i}", [4, 1], F32) for i in range(2)]
            mw = P.sb(es, "mw", [4, 8], F32)
            mrow = P.sb(es, "mrow", [4, 128], F32)
            bc4 = P.sb(es, "bc4", [128, 4], F32)
            pT = P.ps(es, "pTA", [128, 8, 128], BF16)
            pf1 = P.ps(es, "pfA0", [128, 4, 128], F32)
            pt1 = P.ps(es, "ptA0", [128, 512], F32)
            pf1v = T(pf1[:].rearrange("p a b -> p (a b)"), "pf1v")
            pf1v.u = pf1.u
            pt1v = T(pt1[:].rearrange("p (a b) -> p a b", a=4), "pt1v")
            pt1v.u = pt1.u
            pf = [pf1, pt1v]
            pt = [pf1v, pt1]
            pS = P.ps(es, "pS", [128, 512], F32)
            X = [P.ps(es, f"XA{h}", [128, 512], F32) for h in range(4)]
            ATh = [P.sb(es, f"ATh{h}", [128, 128], BF16) for h in range(4)]
            hwh = [P.sb(es, f"hwh{h}", [128, 4], F32) for h in range(4)]
            Cfu = [[T(Cf[si][:, h, :], f"Cfu{si}{h}") for h in range(4)] for si in range(2)]
            Cbu = [[T(Cb[si][:, h, :], f"Cbu{si}{h}") for h in range(4)] for si in range(2)]
            junkh = [T(junk[:, h * 256:(h + 1) * 256], f"junkh{h}") for h in range(4)]

            if peer:
                csA = [P.sb(es, f"cs{i}", [128, 2048], F32) for i in range(3)]
                cbA = [P.sb(es, f"cb{i}", [128, 2048], BF16) for i in range(3)]
                p0.bind(csA, cbA, ["ld0", "ld1", "ld2"])
            P.dma(gm[:], g_mix.rearrange("(c p) -> p c", p=128), [], [gm], "c", allow_slow_non_contiguous=True)
            colsA = [(Q0, OG0 + 1024), (G00, G00 + 1024)]
            P.dma(btok[:, 0:3080], b_in[0:3080].partition_broadcast(128), [], [btok], "c")
            P.dma(btok[:, 3080:4104], b_in[G00:G00 + 1024].partition_broadcast(128), [], [btok], "c")
            P.dma(bq[:], b_in[0:1024].rearrange("(c p) -> p c", p=128), [], [bq], "c", allow_slow_non_contiguous=True)
            P.dma(gmlh[:], g_mlh.partition_broadcast(128), [], [gmlh], "c")
            load_w(es, wA, w_in, colsA, gm, stg)
            if p0 is not None:
                p0.load_some()
            P.memset("dve", vaug[:, :, 256:257], 1.0, [vaug])

            def bcast4(src41, dst):
                P.ts("dve", mrow[:], cst[0:4, C_ONES, :], src41, None, ALU.mult, None, [cst, mw, mst[0], mst[1]],
                     [mrow])
                P.mm([(pS[:, 300:304], mrow[:], cst[0:4, C_IDENT, 0:4], True, True)], [mrow, cst], [pS])
                P.copy("dve", dst, pS[:, 300:304], [pS], [bc4])

            def init_state(si, sample_b=None):
                if sample_b is None:
                    P.memset("dve", Cf[si][:], 0.0, Cfu[si])
                    P.memset("pool", Cb[si][:], 0.0, Cbu[si])
                    P.memset("dve", mst[si][:], 0.0, [mst[si]])
                else:
                    b = sample_b
                    P.dma(Cf[si][:, :, 0:256], sC[b].rearrange("h d v -> d h v"), [], Cfu[si], f"misc{si}")
                    P.dma(Cf[si][:, :, 256:257], sn[b].rearrange("h (d o) -> d h o", o=1), [], Cfu[si], f"misc{si}",
                          allow_slow_non_contiguous=True)
                    P.dma(mst[si][:], sm[b].rearrange("(h o) -> h o", o=1), [], [mst[si]], f"misc{si}",
                          allow_slow_non_contiguous=True)
                    P.act(mw[:, 0:1], mst[si][:], AF.Exp, [mst[si]], [mw])
                    bcast4(mw[:, 0:1], bc4[:])
                    P.tt("dve", Cf[si][:], Cf[si][:], bc4[:].unsqueeze(2).broadcast_to([128, 4, 257]), ALU.mult,
                         Cfu[si] + [bc4], Cfu[si])
                    P.copy("act", Cb[si][:], Cf[si][:], Cfu[si], Cbu[si])

            def final_state(si, C_dram, n_dram, m_dram, b):
                P.act(mw[:, 0:1], mst[si][:], AF.Exp, [mst[si]], [mw], scale=-1.0)
                bcast4(mw[:, 0:1], bc4[:])
                P.tt("dve", Cout[:], Cf[si][:], bc4[:].unsqueeze(2).broadcast_to([128, 4, 257]), ALU.mult,
                     Cfu[si] + [bc4], [Cout])
                P.dma(C_dram[b].rearrange("h d v -> d h v"), Cout[:, :, 0:256], [Cout], [], "oC", eng="pool")
                P.dma(n_dram[b].rearrange("h (d o) -> d h o", o=1), Cout[:, :, 256:257], [Cout], [], "oC", eng="pool",
                      allow_slow_non_contiguous=True)
                P.dma(m_dram[b].rearrange("(h o) -> h o", o=1), mst[si][:], [mst[si]], [], "oC", eng="pool",
                      allow_slow_non_contiguous=True)

            LNK = math.log(128 ** -0.5)
            mahs = {}
            for ti in range(NT):
                sample = (ti == NT - 1)
                x = xt[ti % 2]
                xT = xnT[ti % 2]
                ma = mat[ti % 2]
                if sample:
                    init_state(0, 0)
                    init_state(1, 1)
                    segs = [(0, C_R0, 0, 64), (1, C_R1, 64, 128)]
                elif ti % NTP == 0:
                    init_state(0)
                    segs = [(0, C_ONES, 0, 128)]
                else:
                    segs = [(0, C_ONES, 0, 128)]
                mask = cst[:, C_TRIUS if sample else C_TRIU, :]
                if ti == 0:
                    P.dma(x[:], xall[0:128, :], [], [x], "x0")
                if ti + 1 < NT:
                    xnx = xt[(ti + 1) % 2]
                    P.dma(xnx[:], xall[(ti + 1) * 128:(ti + 2) * 128, :], [], [xnx], f"x{(ti+1)%2}")
                rmsnorm_T(es, x[:], x, junk, ss, rstd, xn, pT, xT)
                P.dma(xnT_d[ti].rearrange("p (c t) -> p c t", c=8), xT[:], [xT], [xnT_d], f"pst{ti%2}", eng="pool")
                for j, (dst, c0, wo) in enumerate(((qT, 0, oQ), (kT, 4, oK))):
                    pp = pf[j]
                    for h in range(4):
                        P.mm([(pp[:, h, :], wA[:, c, wo + h * 128:wo + (h + 1) * 128], xT[:, c, :], c == 0, c == 7)
                              for c in range(8)], [wA, xT], [pp])
                    P.tt("dve", dst[:], pp[:], bq[:, c0:c0 + 4].unsqueeze(2).broadcast_to([128, 4, 128]), ALU.add,
                         [pp, bq], [dst])
                pi = [0]

                def tok_block(wo, n, dst_ap, dst_t, eng="dve"):
                    pp = pt[pi[0] % 2]
                    pi[0] += 1
                    P.mm([(pp[:, 0:n], xT[:, c, :], wA[:, c, wo:wo + n], c == 0, c == 7) for c in range(8)],
                         [xT, wA], [pp])
                    P.tt(eng, dst_ap, pp[:, 0:n], btok[:, wo:wo + n], ALU.add, [pp, btok], [dst_t])

                tok_block(oIG, 8, gsb[:], gsb)
                tok_block(oK, 512, ktok[:], ktok)
                for hh in range(2):
                    pp = pt[pi[0] % 2]
                    pi[0] += 1
                    wo = oV + hh * 512
                    P.mm([(pp[:], xT[:, c, :], wA[:, c, wo:wo + 512], c == 0, c == 7) for c in range(8)],
                         [xT, wA], [pp])
                    P.tt("dve", vaug[:, 2 * hh:2 * hh + 2, 0:256], pp[:].rearrange("p (h v) -> p h v", h=2),
                         btok[:, wo:wo + 512].rearrange("p (h v) -> p h v", h=2), ALU.add, [pp, btok], [vaug])
                for hh in range(2):
                    tok_block(oOG + hh * 512, 512, gateA[:, hh * 512:(hh + 1) * 512], gateA)
                for hh in range(2):
                    tok_block(oG0 + hh * 512, 512, g0s[:, hh * 512:(hh + 1) * 512], g0s)
                P.act(gateA[:], gateA[:], AF.Sigmoid, [gateA], [gateA])
                P.act(g0s[:], g0s[:], AF.Sigmoid, [g0s], [g0s])
                P.tt("dve", gateA[:], gateA[:], g0s[:], ALU.mult, [gateA, g0s], [gateA])
                P.tt("dve", gateA[:], gateA[:], gmlh[:], ALU.mult, [gateA, gmlh], [gateA])
                P.act(gw[:, 0:4], gsb[:, 4:8], AF.Exp, [gsb], [gw], scale=-1.0)
                P.act(gw[:, 0:4], gw[:, 0:4], AF.Ln, [gw], [gw], bias=1.0)
                lst = [(pS[:, 256:260], mask, gw[:, 0:4], True, True)]
                for k, (si, rc, lo, hi) in enumerate(segs):
                    lst.append((pS[:, 260 + 4 * k:264 + 4 * k], cst[:, rc, :], gw[:, 0:4], True, True))
                P.mm(lst, [cst, gw], [pS])
                nsg = 4 * len(segs)
                P.copy("dve", gw[:, 4:8 + nsg], pS[:, 256:260 + nsg], [pS], [gw])
                P.tt("dve", gw[:, 16:20], gsb[:, 0:4], gw[:, 4:8], ALU.add, [gsb, gw], [gw])
                P.act(gw[:, 20:24], gw[:, 16:20], AF.Exp, [gw], [gw], bias=LNK)
                P.act(gw[:, 24:28], gw[:, 4:8], AF.Exp, [gw], [gw])
                P.act(gw[:, 28:28 + nsg], gw[:, 8:8 + nsg], AF.Exp, [gw], [gw], scale=-1.0)
                P.tr([(pS[0:4, 384:512], gw[:, 16:20], identf)], [gw, cst], [pS])
                P.copy("dve", mrow[:], pS[0:4, 384:512], [pS], [mrow])
                for k, (si, rc, lo, hi) in enumerate(segs):
                    P.add("dve", lambda e, lo=lo, hi=hi: e.reduce_max(out=mw[:, 1:2], in_=mrow[:, lo:hi], axis=AX.X),
                          [mrow], [mw])
                    P.tt("dve", mw[:, 1:2], mw[:, 1:2], mst[si][:], ALU.max, [mw, mst[si]], [mw])
                    P.tr([(pS[0:4, 384:512], gw[:, 8 + 4 * k:12 + 4 * k], identf)], [gw, cst], [pS])
                    P.tt("dve", mst[si][:], mw[:, 1:2], pS[0:4, 384 + lo:385 + lo], ALU.subtract, [mw, pS], [mst[si]])
                for k, (si, rc, lo, hi) in enumerate(segs):
                    for h in range(4):
                        P.ts("dve", kts[k][:, h * 128:(h + 1) * 128], ktok[:, h * 128:(h + 1) * 128],
                             gw[:, 20 + h:21 + h], None, ALU.mult, None, [ktok, gw], [kts[k]])
                    if sample:
                        P.ts("pool", kts[k][:], kts[k][:], cst[:, C_MISC, k:k + 1], None, ALU.mult, None,
                             [kts[k], cst], [kts[k]])
                        P.tt("pool", qTm[k][:], qT[:], cst[:, C_HM0ROW + k, :].unsqueeze(1).broadcast_to([128, 4, 128]),
                             ALU.mult, [qT, cst], [qTm[k]])
                mah = [T(ma[:, h * 256:(h + 1) * 256], f"mah{ti%2}{h}") for h in range(4)] if ti < 2 else mahs[ti % 2]
                mahs[ti % 2] = mah
                H4 = range(4)
                for h in H4:
                    P.mm([(X[h][:, 0:128], kT[:, h, :], qT[:, h, :], True, True)], [kT, qT], [X[h]])
                for h in H4:
                    P.stt("dve", ATh[h][:], X[h][:, 0:128], gw[:, 20 + h:21 + h], mask, ALU.mult, ALU.mult,
                          [X[h], gw, cst], [ATh[h]])
                for h in H4:
                    lst = [(X[h][:, 128:385], ATh[h][:], vaug[:, h, :], True, False)]
                    rd = [ATh[h], vaug, qT]
                    for k, (si, rc, lo, hi) in enumerate(segs):
                        qsrc = qTm[k] if sample else qT
                        lst.append((X[h][:, 128:385], qsrc[:, h, :], Cb[si][:, h, :], False, k == len(segs) - 1))
                        rd += [qsrc, Cbu[si][h]]
                    P.mm(lst, rd, [X[h]])
                for h in H4:
                    P.act(hwh[h][:, 0:1], X[h][:, 384:385], AF.Abs, [X[h]], [hwh[h]])
                for h in H4:
                    P.tt("dve", hwh[h][:, 0:1], hwh[h][:, 0:1], gw[:, 24 + h:25 + h], ALU.max, [hwh[h], gw], [hwh[h]])
                for h in H4:
                    P.recip(hwh[h][:, 0:1], hwh[h][:, 0:1], [hwh[h]], [hwh[h]])
                for h in H4:
                    P.act(junkh[h][:], X[h][:, 128:384], AF.Square, [X[h], hwh[h]], [junkh[h], hwh[h]],
                          scale=hwh[h][:, 0:1], accum=hwh[h][:, 1:2])
                for h in H4:
                    P.act(hwh[h][:, 2:3], hwh[h][:, 1:2], AF.Sqrt, [hwh[h]], [hwh[h]], bias=EPS, scale=1.0 / 256)
                for h in H4:
                    P.recip(hwh[h][:, 2:3], hwh[h][:, 2:3], [hwh[h]], [hwh[h]])
                for h in H4:
                    P.tt("dve", hwh[h][:, 3:4], hwh[h][:, 2:3], hwh[h][:, 0:1], ALU.mult, [hwh[h]], [hwh[h]])
                for h in H4:
                    P.stt("dve", ma[:, h * 256:(h + 1) * 256], X[h][:, 128:384], hwh[h][:, 3:4],
                          gateA[:, h * 256:(h + 1) * 256], ALU.mult, ALU.mult, [X[h], hwh[h], gateA], [mah[h]])
                for k, (si, rc, lo, hi) in enumerate(segs):
                    for h in H4:
                        P.mm([(X[h][:, 128:385], kts[k][:, h * 128:(h + 1) * 128], vaug[:, h, :], True, True)],
                             [kts[k], vaug], [X[h]])
                    for h in H4:
                        eb = gw[:, 28 + 4 * k + h:29 + 4 * k + h]
                        P.ts("dve", Cf[si][:, h, :], Cf[si][:, h, :], eb, None, ALU.mult, None, [Cfu[si][h], gw],
                             [Cfu[si][h]])
                    for h in H4:
                        eb = gw[:, 28 + 4 * k + h:29 + 4 * k + h]
                        P.stt("dve", Cf[si][:, h, :], X[h][:, 128:385], eb, Cf[si][:, h, :], ALU.mult, ALU.add,
                              [X[h], gw, Cfu[si][h]], [Cfu[si][h]])
                    for h in H4:
                        P.copy("act", Cb[si][:, h, :], Cf[si][:, h, :], [Cfu[si][h]], [Cbu[si][h]])
                P.dma(ma_d[ti * 128:(ti + 1) * 128, :], ma[:], mah, [ma_d], f"o{ti%2}", eng="pool")
                if p0 is not None:
                    p0.step()
                if sample:
                    final_state(0, Cs_o, ns_o, ms_o, 0)
                    final_state(1, Cs_o, ns_o, ms_o, 1)
                elif ti % NTP == NTP - 1:
                    final_state(0, Cp_o, np_o, mp_o, ti // NTP)
            if p0 is not None:
                p0.cast_some()
            P.phase_end()

        with ExitStack() as es:
            NB = 3072
            oSU, oSV, oG1 = 0, 1024, 2048
            gm = P.sb(es, "gmB", [128, 8], F32)
            stg = [P.sb(es, f"stgB{i}", [128, 1024], F32) for i in range(2)]
            wB = P.sb(es, "wB", [128, 8, NB], BF16)
            btok = P.sb(es, "btokB", [128, NB], F32)
            gsgu = P.sb(es, "gsgu", [128, 1024], F32)
            wsT = [P.sb(es, f"wsT{i}", [128, 4, 128], BF16) for i in range(2)]
            wsl = [P.sb(es, f"wsl{i}", [128, 4, 128], F32) for i in range(2)]
            ltri = P.sb(es, "ltri", [128, 2, 128], F32)
            bs = [P.sb(es, f"bs{i}", [128, 4], F32) for i in range(2)]
            xT = [P.sb(es, f"xTB{i}", [128, 8, 128], BF16) for i in range(3)]
            mat = [P.sb(es, f"maB{i}", [128, 1024], F32) for i in range(3)]
            kxT = ["st0", "st1", "x0"]
            kma = ["ld2", "ld3", "x1"]
            gv = P.sb(es, "gv", [128, 1024], F32)
            g1 = P.sb(es, "g1", [128, 1024], F32)
            vn = [P.sb(es, f"vn{i}", [128, 1024], F32) for i in range(2)]
            vnb2 = [P.sb(es, f"vnb{i}", [128, 1024], BF16) for i in range(2)]
            ub2 = [P.sb(es, f"ub2{i}", [128, 1024], F32) for i in range(2)]
            junk = P.sb(es, "junkB", [128, 1024], F32)
            junk2 = P.sb(es, "junkB2", [128, 1024], F32)
            sw = P.sb(es, "sw", [128, 16], F32)
            mhalf = P.sb(es, "mhalfB", [128, 1], F32)
            P.memset("dve", mhalf[:], -0.5, [mhalf])
            pt = [P.ps(es, f"ptB{i}", [128, 512], F32) for i in range(2)]
            pa = [P.ps(es, f"paB{i}", [128, 512], F32) for i in range(2)]

            if p0 is not None:
                csB = [P.sb(es, f"csB{i}", [128, 2048], F32) for i in range(3)]
                cbB = [P.sb(es, f"cbB{i}", [128, 2048], BF16) for i in range(3)]
                p0.bind(csB, cbB, ["ld0", "ld1", "misc1"])
            P.dma(gm[:], g_mix.rearrange("(c p) -> p c", p=128), [], [gm], "c", allow_slow_non_contiguous=True)
            P.dma(btok[:, 0:2048], b_in[SU0:XQ0].partition_broadcast(128), [], [btok], "c")
            P.dma(btok[:, 2048:3072], b_in[G10:G20].partition_broadcast(128), [], [btok], "c")
            P.dma(gsgu[:], g_sgu.partition_broadcast(128), [], [gsgu], "c")
            P.dma(wsl[0][:], w_s.rearrange("g t s -> t g s"), [], [wsl[0]], "c")
            P.dma(bs[0][:], b_s.rearrange("g t -> t g"), [], [bs[0]], "c", allow_slow_non_contiguous=True)
            P.dma(bs[1][0:64, :], b_s[:, 0:64].rearrange("g t -> t g"), [], [bs[1]], "c", allow_slow_non_contiguous=True)
            P.dma(bs[1][64:128, :], b_s[:, 0:64].rearrange("g t -> t g"), [], [bs[1]], "c",
                  allow_slow_non_contiguous=True)
            P.memset("dve", wsl[1][:], 0.0, [wsl[1]])
            P.dma(wsl[1][0:64, :, 0:64], w_s[:, 0:64, 0:64].rearrange("g t s -> t g s"), [], [wsl[1]], "misc0")
            P.dma(wsl[1][64:128, :, 64:128], w_s[:, 0:64, 0:64].rearrange("g t s -> t g s"), [], [wsl[1]], "misc0")
            load_w(es, wB, w_in, [(SU0, XQ0), (G10, G20)], gm, stg)
            P.tr([(pt[0][:, 0:128], cst[:, C_TRIU, :], identf), (pt[0][:, 128:256], cst[:, C_TRIUS, :], identf)],
                 [cst], [pt[0]])
            P.copy("dve", ltri[:].rearrange("p a b -> p (a b)"), pt[0][:, 0:256], [pt[0]], [ltri])
            for i in range(2):
                P.tt("dve", wsl[i][:], wsl[i][:], ltri[:, i, :].unsqueeze(1).broadcast_to([128, 4, 128]), ALU.mult,
                     [wsl[i], ltri], [wsl[i]])
                P.tr([(pa[i][:, g * 128:(g + 1) * 128], wsl[i][:, g, :], identf) for g in range(4)], [wsl[i], cst],
                     [pa[i]])
                P.copy("dve", wsT[i][:].rearrange("p g t -> p (g t)"), pa[i][:], [pa[i]], [wsT[i]])

            def frontB(ti):
                sample = (ti == NT - 1)
                xTt = xT[ti % 3]
                ma = mat[ti % 3]
                vnt = vn[ti % 2]
                ut = ub2[ti % 2]
                vb_ = vnb2[ti % 2]
                P.dma(xTt[:], xnT_d[ti].rearrange("p (c t) -> p c t", c=8), [xnT_d], [xTt], kxT[ti % 3])
                P.dma(ma[:], ma_d[ti * 128:(ti + 1) * 128, :], [ma_d], [ma], kma[ti % 3])
                pi = [0]

                def tok_block(wo_, n, dst_ap, dst_t, eng="dve"):
                    pp = pt[pi[0] % 2]
                    pi[0] += 1
                    P.mm([(pp[:, 0:n], xTt[:, c, :], wB[:, c, wo_:wo_ + n], c == 0, c == 7) for c in range(8)],
                         [xTt, wB], [pp])
                    P.tt(eng, dst_ap, pp[:, 0:n], btok[:, wo_:wo_ + n], ALU.add, [pp, btok], [dst_t])

                for hh in range(2):
                    tok_block(oSV + hh * 512, 512, gv[:, hh * 512:(hh + 1) * 512], gv)
                P.act(gv[:], gv[:], AF.Gelu_apprx_tanh, [gv], [gv])
                P.act(junk2[:], gv[:], AF.Square, [gv], [junk2, sw], accum=sw[:, 0:1])
                P.ts("dve", sw[:, 2:3], sw[:, 0:1], 1.0 / 1024, EPS, ALU.mult, ALU.add, [sw], [sw])
                P.tt("pool", sw[:, 1:2], sw[:, 2:3], mhalf[:], ALU.pow, [sw, mhalf], [sw])
                for hh in range(2):
                    tok_block(oSU + hh * 512, 512, ut[:, hh * 512:(hh + 1) * 512], ut)
                for hh in range(2):
                    tok_block(oG1 + hh * 512, 512, g1[:, hh * 512:(hh + 1) * 512], g1)
                P.stt("dve", vnt[:], gv[:], sw[:, 1:2], gsgu[:], ALU.mult, ALU.mult, [gv, sw, gsgu], [vnt])
                P.copy("act", vb_[:], vnt[:], [vnt], [vb_])
                if sample:
                    P.dma(sguv_o, vnt[:], [vnt], [], "oC", eng="pool")
                P.act(ut[:], ut[:], AF.Gelu_apprx_tanh, [ut], [ut])
                P.act(g1[:], g1[:], AF.Tanh, [g1], [g1], scale=0.5)
                P.ts("dve", g1[:], g1[:], 0.5, 0.5, ALU.mult, ALU.add, [g1], [g1])
                P.tt("dve", ut[:], ut[:], g1[:], ALU.mult, [ut, g1], [ut])

            def backB(ti):
                sample = (ti == NT - 1)
                ma = mat[ti % 3]
                ut = ub2[ti % 2]
                vb_ = vnb2[ti % 2]
                sv_i = 1 if sample else 0
                for g in range(4):
                    pp = pa[g % 2]
                    P.mm([(pp[:, 0:256], wsT[sv_i][:, g, :], vb_[:, g * 256:(g + 1) * 256], True, True)],
                         [wsT[sv_i], vb_], [pp])
                    P.stt("dve", junk[:, g * 256:(g + 1) * 256], pp[:, 0:256], bs[sv_i][:, g:g + 1],
                          ut[:, g * 256:(g + 1) * 256], ALU.add, ALU.mult, [pp, bs[sv_i], ut], [junk])
                P.tt("dve", ma[:], ma[:], junk[:], ALU.add, [ma, junk], [ma])
                P.dma(ma_d[ti * 128:(ti + 1) * 128, :], ma[:], [ma], [ma_d], f"o{ti%2}", eng="pool")
                if p0 is not None:
                    p0.step()

            if p0 is not None:
                p0.load_some()
            if PIPE_B:
                frontB(0)
                for ti in range(NT):
                    if ti + 1 < NT:
                        frontB(ti + 1)
                    backB(ti)
            else:
                for ti in range(NT):
                    frontB(ti)
                    backB(ti)
            if p0 is not None:
                while p0.pending or p0.next_piece < 128:
                    p0.step()
            P.phase_end()

        with ExitStack() as es:
            NCW = 2048
            oXQ, oG2 = 0, 1024
            gm = P.sb(es, "gmC", [128, 8], F32)
            ones8 = P.sb(es, "ones8", [128, 8], F32)
            stg = [P.sb(es, f"stgC{i}", [128, 1024], F32) for i in range(2)]
            wC = P.sb(es, "wC", [128, 8, NCW], BF16)
            wo = P.sb(es, "wo", [128, 8, 1024], BF16)
            bxq = P.sb(es, "bxq", [128, 8], F32)
            btok = P.sb(es, "btokC", [128, 1024], F32)
            gfin = P.sb(es, "gfin", [128, 1024], F32)
            KT = [P.sb(es, f"KT{i}", [128, 8, 256], BF16) for i in range(3)]
            Vb = [P.sb(es, f"Vb{i}", [128, 2, 1024], BF16) for i in range(3)]
            kvs = [P.sb(es, f"kvs{i}", [128, 1024], F32) for i in range(2)]
            kvb = P.sb(es, "kvb", [128, 1024], BF16)
            xT = [P.sb(es, f"xTC{i}", [128, 8, 128], BF16) for i in range(3)]
            xt = [P.sb(es, f"xC{i}", [128, 1024], F32) for i in range(3)]
            mat = [P.sb(es, f"maC{i}", [128, 1024], F32) for i in range(3)]
            kx = ["x0", "x1", "misc0"]
            kxT = ["st0", "st1", "misc1"]
            kma = ["ld2", "ld3", "ws0"]
            junk = P.sb(es, "junkC", [128, 1024], F32)
            sw = P.sb(es, "swC", [128, 16], F32)
            xqTb = [P.sb(es, f"xqT{i}", [128, 8, 128], BF16) for i in range(2)]
            g2b = [P.sb(es, f"g2{i}", [128, 1024], F32) for i in range(2)]
            prh = [P.sb(es, f"prh{i}", [128, 256], BF16) for i in range(4)]
            prTall = P.sb(es, "prTall", [128, 8, 128], BF16)
            sth = [P.sb(es, f"sth{i}", [128, 2], F32) for i in range(4)]
            sth2 = [P.sb(es, f"sth2{i}", [128, 2], F32) for i in range(4)]
            hc = P.sb(es, "hc", [128, 1024], F32)
            hc2 = P.sb(es, "hc2", [128, 1024], F32)
            pr = P.sb(es, "pr", [128, 256], BF16)
            prT = P.sb(es, "prT", [128, 2, 128], BF16)
            mg = P.sb(es, "mg", [128, 1024], BF16)
            mgT = P.sb(es, "mgT", [128, 8, 128], BF16)
            x2 = [P.sb(es, f"x2{i}", [128, 1024], F32) for i in range(2)]
            yt = [P.sb(es, f"yt{i}", [128, 1024], F32) for i in range(2)]
            pT = P.ps(es, "pTC", [128, 8, 128], BF16)
            pf = P.ps(es, "pfC", [128, 4, 128], F32)
            pt = [P.ps(es, f"ptC{i}", [128, 512], F32) for i in range(2)]
            pa = [P.ps(es, f"paC{i}", [128, 512], F32) for i in range(4)]
            po2 = [pt[0], pt[1]]

            P.dma(gm[:], g_mix.rearrange("(c p) -> p c", p=128), [], [gm], "c", allow_slow_non_contiguous=True)
            P.dma(btok[:], b_in[G20:DIN].partition_broadcast(128), [], [btok], "c")
            P.dma(bxq[:], b_in[XQ0:XQ0 + 1024].rearrange("(c p) -> p c", p=128), [], [bxq], "c",
                  allow_slow_non_contiguous=True)
            P.dma(gfin[:], g_final.partition_broadcast(128), [], [gfin], "c")
            P.memset("dve", ones8[:], 1.0, [ones8])
            load_w(es, wC, w_in, [(XQ0, G00), (G20, DIN)], gm, stg)
            load_w(es, wo, w_out, [(0, 1024)], ones8, stg)

            def load_kv(slot, k_dram, v_dram, k_u, v_u):
                for mc in range(2):
                    s_ = kvs[mc % 2]
                    P.dma(s_[:], k_dram[mc * 128:(mc + 1) * 128, :], [k_u] if k_u else [], [s_], f"ld{mc}")
                    P.copy("dve", kvb[:], s_[:], [s_], [kvb])
                    P.tr([(pT[:, c, :], kvb[:, c * 128:(c + 1) * 128], identb[:]) for c in range(8)], [kvb, identb], [pT])
                    P.copy("act", KT[slot][:, :, mc * 128:(mc + 1) * 128], pT[:], [pT], [KT[slot]])
                for mc in range(2):
                    s_ = kvs[mc % 2]
                    P.dma(s_[:], v_dram[mc * 128:(mc + 1) * 128, :], [v_u] if v_u else [], [s_], f"ld{mc}")
                    P.copy("act", Vb[slot][:, mc, :], s_[:], [s_], [Vb[slot]])

            def frontC(ti):
                sample = (ti == NT - 1)
                x = xt[ti % 3]
                xTt = xT[ti % 3]
                ma = mat[ti % 3]
                g2 = g2b[ti % 2]
                xqT = xqTb[ti % 2]
                if sample:
                    load_kv(0, cmk[0], cmv[0], None, None)
                    load_kv(2, cmk[1], cmv[1], None, None)
                elif ti % NTP == 0:
                    b = ti // NTP
                    load_kv(b, mk_o[b], mv_o[b], mk_u, mv_u)
                P.dma(x[:], xall[ti * 128:(ti + 1) * 128, :], [], [x], kx[ti % 3])
                P.dma(xTt[:], xnT_d[ti].rearrange("p (c t) -> p c t", c=8), [xnT_d], [xTt], kxT[ti % 3])
                P.dma(ma[:], ma_d[ti * 128:(ti + 1) * 128, :], [ma_d], [ma], kma[ti % 3])
                for hh in range(2):
                    pp = pt[hh]
                    P.mm([(pp[:], xTt[:, c, :], wC[:, c, oG2 + hh * 512:oG2 + (hh + 1) * 512], c == 0, c == 7)
                          for c in range(8)], [xTt, wC], [pp])
                    P.tt("dve", g2[:, hh * 512:(hh + 1) * 512], pp[:], btok[:, hh * 512:(hh + 1) * 512], ALU.add,
                         [pp, btok], [g2])
                P.act(g2[:], g2[:], AF.Tanh, [g2], [g2], scale=0.5)
                P.ts("dve", g2[:], g2[:], 0.5, 0.5, ALU.mult, ALU.add, [g2], [g2])
                for j in range(2):
                    for h in range(4):
                        cc = j * 4 + h
                        P.mm([(pf[:, h, :], wC[:, c, oXQ + cc * 128:oXQ + (cc + 1) * 128], xTt[:, c, :], c == 0, c == 7)
                              for c in range(8)], [wC, xTt], [pf])
                    P.tt("dve", xqT[:, j * 4:(j + 1) * 4, :], pf[:],
                         bxq[:, j * 4:(j + 1) * 4].unsqueeze(2).broadcast_to([128, 4, 128]), ALU.add, [pf, bxq], [xqT])

            def attention(dst, slot, g2, xqT):
                for h in range(4):
                    P.mm([(pa[h][:, 0:256], xqT[:, 2 * h + dc, :], KT[slot][:, 2 * h + dc, :], dc == 0, dc == 1)
                          for dc in range(2)], [xqT, KT[slot]], [pa[h]])
                for h in range(4):
                    P.add("dve", lambda e, h=h: e.reduce_max(out=sth[h][:, 0:1], in_=pa[h][:, 0:256], axis=AX.X),
                          [pa[h]], [sth[h]])
                for h in range(4):
                    P.ts("dve", sth[h][:, 1:2], sth[h][:, 0:1], -1.0 / 16, None, ALU.mult, None, [sth[h]], [sth[h]])
                for h in range(4):
                    P.act(prh[h][:], pa[h][:, 0:256], AF.Exp, [pa[h], sth[h]], [prh[h], sth2[h]], bias=sth[h][:, 1:2],
                          scale=1.0 / 16, accum=sth2[h][:, 0:1])
                for h in range(4):
                    P.recip(sth2[h][:, 1:2], sth2[h][:, 0:1], [sth2[h]], [sth2[h]])
                P.tr([(pT[:, 2 * h + mc, :], prh[h][:, mc * 128:(mc + 1) * 128], identb[:])
                      for h in range(4) for mc in range(2)], prh + [identb], [pT])
                P.copy("act", prTall[:], pT[:], [pT], [prTall])
                for h in range(4):
                    P.mm([(pa[h][:, 256:512], prTall[:, 2 * h + mc, :], Vb[slot][:, mc, h * 256:(h + 1) * 256],
                           mc == 0, mc == 1) for mc in range(2)], [prTall, Vb[slot]], [pa[h]])
                for h in range(4):
                    P.stt("dve", dst[:, h * 256:(h + 1) * 256], pa[h][:, 256:512], sth2[h][:, 1:2],
                          g2[:, h * 256:(h + 1) * 256], ALU.mult, ALU.mult, [pa[h], sth2[h], g2], [dst])

            def backC(ti):
                sample = (ti == NT - 1)
                x = xt[ti % 3]
                ma = mat[ti % 3]
                g2 = g2b[ti % 2]
                xqT = xqTb[ti % 2]
                if sample:
                    attention(hc, 0, g2, xqT)
                    attention(hc2, 2, g2, xqT)
                    P.ts("pool", hc[:], hc[:], cst[:, C_MISC, 0:1], None, ALU.mult, None, [hc, cst], [hc])
                    P.stt("dve", hc[:], hc2[:], cst[:, C_MISC, 1:2], hc[:], ALU.mult, ALU.add, [hc2, cst, hc], [hc])
                else:
                    attention(hc, ti // NTP, g2, xqT)
                P.tt("dve", mg[:], ma[:], hc[:], ALU.add, [ma, hc], [mg])
                P.tr([(pT[:, c, :], mg[:, c * 128:(c + 1) * 128], identb[:]) for c in range(8)], [mg, identb], [pT])
                P.copy("act", mgT[:], pT[:], [pT], [mgT])
                xo = x2[ti % 2]
                for hh in range(2):
                    pp = po2[hh]
                    P.mm([(pp[:], mgT[:, c, :], wo[:, c, hh * 512:(hh + 1) * 512], c == 0, c == 7) for c in range(8)],
                         [mgT, wo], [pp])
                    P.tt("dve", xo[:, hh * 512:(hh + 1) * 512], pp[:], x[:, hh * 512:(hh + 1) * 512], ALU.add,
                         [pp, x], [xo])
                P.dma(x2_d[ti * 128:(ti + 1) * 128, :], xo[:], [xo], [x2_d], f"o{ti%2}", eng="pool")
                if not peer:
                    yo = yt[ti % 2]
                    P.act(junk[:], xo[:], AF.Square, [xo], [junk, sw], accum=sw[:, 8:9])
                    P.act(sw[:, 9:10], sw[:, 8:9], AF.Sqrt, [sw], [sw], bias=EPS, scale=1.0 / 1024)
                    P.recip(sw[:, 9:10], sw[:, 9:10], [sw], [sw])
                    P.stt("dve", yo[:], xo[:], sw[:, 9:10], gfin[:], ALU.mult, ALU.mult, [xo, sw, gfin], [yo])
                    P.dma(y_o[ti * 128:(ti + 1) * 128, :], yo[:], [yo], [], f"sc{ti%2}", eng="pool")

            if PIPE_C:
                frontC(0)
                for ti in range(NT):
                    if ti + 1 < NT:
                        frontC(ti + 1)
                    backC(ti)
            else:
                for ti in range(NT):
                    frontC(ti)
                    backC(ti)
            P.phase_end(final=not peer)

        if not peer:
            return nc

        xn2T_d = T(dscr("xn2T_d", [NT, 128, 1024], BF16), "xn2T_d")
        rt_d = T(dscr("rt_d", [NT, 128, 384]), "rt_d")

        with ExitStack() as es:
            ones8 = P.sb(es, "ones8p", [128, 8], F32)
            stg = [P.sb(es, f"stgP{i}", [128, 1024], F32) for i in range(2)]
            wpq = P.sb(es, "wpq", [128, 8, 2048], BF16)
            gffn = P.sb(es, "gffn", [128, 1024], F32)
            ks = P.sb(es, "ks", [128, 2, 128], F32)
            ksT = P.sb(es, "ksT", [128, 2, 128], F32)
            xt = [P.sb(es, f"xP{i}", [128, 1024], F32) for i in range(3)]
            kxp = ["x0", "x1", "misc0"]
            junk = P.sb(es, "junkP", [128, 1024], F32)
            sw = P.sb(es, "swP", [128, 8], F32)
            mhalfP = P.sb(es, "mhalfP", [128, 1], F32)
            P.memset("dve", mhalfP[:], -0.5, [mhalfP])
            xn = P.sb(es, "xnP", [128, 1024], BF16)
            xT = [P.sb(es, f"xTP{i}", [128, 8, 128], BF16) for i in range(2)]
            qT = P.sb(es, "qTP", [128, 16, 128], F32)
            Sb = [P.sb(es, f"S{i}", [128, 16, 128], F32) for i in range(2)]
            xn32 = P.sb(es, "xn32", [128, 1024], F32)
            Sw = P.sb(es, "Sw", [128, 16, 128], F32)
            Sw2 = P.sb(es, "Sw2", [128, 8, 256], F32)
            V = P.sb(es, "V", [128, 16, 16], F32)
            I = P.sb(es, "I", [128, 16, 16], U32)
            Sub = [[T(Sb[k][:, 4 * i:4 * i + 4, :], f"Su{k}{i}") for i in range(4)] for k in range(2)]
            Vu = [T(V[:, j, :], f"Vu{j}") for j in range(16)]
            Iu = [T(I[:, j, :], f"Iu{j}") for j in range(16)]
            Swu = [T(Sw[:, j, :], f"Swu{j}") for j in range(16)]
            If = P.sb(es, "If", [128, 16, 16], F32)
            cand = P.sb(es, "cand", [128, 8, 256], F32)
            scv = P.sb(es, "scv", [128, 8, 16], F32)
            pos = P.sb(es, "pos", [128, 8, 16], U32)
            scu = [T(scv[:, h, :], f"scu{h}") for h in range(8)]
            posu = [T(pos[:, h, :], f"posu{h}") for h in range(8)]
            Sw2u = [T(Sw2[:, h, :], f"Sw2u{h}") for h in range(8)]
            pa_ = P.sb(es, "pa_", [128, 8, 16], U32)
            pb_ = P.sb(es, "pb_", [128, 8, 16], U32)
            af = P.sb(es, "af", [128, 8, 16], F32)
            bf = P.sb(es, "bf", [128, 8, 16], F32)
            oh = P.sb(es, "oh", [128, 8, 16, 16], F32)
            oh2 = P.sb(es, "oh2", [128, 8, 16, 16], F32)
            ex = P.sb(es, "ex", [128, 8, 16], F32)
            sm8 = P.sb(es, "sm8", [128, 8], F32)
            res = P.sb(es, "res", [128, 3, 128], F32)
            rt = [P.sb(es, f"rt{i}", [128, 3, 128], F32) for i in range(2)]
            pT = P.ps(es, "pTP", [128, 8, 128], BF16)
            pq = [P.ps(es, f"pq{i}", [128, 4, 128], F32) for i in range(4)]
            ptr = P.ps(es, "ptr", [128, 3, 128], F32)

            P.dma(gffn[:], g_ffn.partition_broadcast(128), [], [gffn], "c")
            P.dma(ks[:, 0, :], k_sub1, [], [ks], "c")
            P.dma(ks[:, 1, :], k_sub2, [], [ks], "c")
            P.memset("dve", ones8[:], 1.0, [ones8])
            load_w(es, wpq, w_pq, [(0, 2048)], ones8, stg)
            P.tr([(ptr[:, i, :], ks[:, i, :], identf) for i in range(2)], [ks, cst], [ptr])
            P.copy("dve", ksT[:], ptr[:, 0:2, :], [ptr], [ksT])
            NEG = -1.0e30

            def top16_rows(rows):
                for (src, su, wu, vu, iu) in rows:
                    P.add("dve", lambda e, src=src, vu=vu: e.max(out=vu[:, 0:8], in_=src), [su], [vu])
                for (src, su, wu, vu, iu) in rows:
                    P.add("dve", lambda e, src=src, vu=vu, iu=iu: e.max_index(out=iu[:, 0:8], in_max=vu[:, 0:8],
                                                                            in_values=src), [su, vu], [iu])
                for (src, su, wu, vu, iu) in rows:
                    P.add("dve", lambda e, src=src, vu=vu, wu=wu: e.match_replace(
                        out=wu[:], in_to_replace=vu[:, 0:8], in_values=src, imm_value=NEG), [su, vu], [wu])
                for (src, su, wu, vu, iu) in rows:
                    P.add("dve", lambda e, vu=vu, wu=wu: e.max(out=vu[:, 8:16], in_=wu[:]), [wu], [vu])
                for (src, su, wu, vu, iu) in rows:
                    P.add("dve", lambda e, vu=vu, wu=wu, iu=iu: e.max_index(out=iu[:, 8:16], in_max=vu[:, 8:16],
                                                                           in_values=wu[:]), [wu, vu], [iu])

            def front(ti):
                x = xt[ti % 3]
                xTt = xT[ti % 2]
                S = Sb[ti % 2]
                Su = Sub[ti % 2]
                P.dma(x[:], x2_d[ti * 128:(ti + 1) * 128, :], [x2_d], [x], kxp[ti % 3])
                P.act(junk[:], x[:], AF.Square, [x], [junk, sw], accum=sw[:, 0:1])
                P.ts("dve", sw[:, 2:3], sw[:, 0:1], 1.0 / 1024, EPS, ALU.mult, ALU.add, [sw], [sw])
                P.tt("pool", sw[:, 1:2], sw[:, 2:3], mhalfP[:], ALU.pow, [sw, mhalfP], [sw])
                P.ts("pool", xn32[:], x[:], sw[:, 1:2], None, ALU.mult, None, [x, sw], [xn32])
                P.tt("pool", xn[:], xn32[:], gffn[:], ALU.mult, [xn32, gffn], [xn])
                P.tr([(pT[:, c, :], xn[:, c * 128:(c + 1) * 128], identb[:]) for c in range(8)], [xn, identb], [pT])
                P.copy("act", xTt[:], pT[:], [pT], [xTt])
                P.dma(xn2T_d[ti].rearrange("p (c t) -> p c t", c=8), xTt[:], [xTt], [xn2T_d], f"pst{ti%2}", eng="pool")
                for bnk in range(4):
                    for jj in range(4):
                        j = bnk * 4 + jj
                        P.mm([(pq[bnk][:, jj, :], wpq[:, c, j * 128:(j + 1) * 128], xTt[:, c, :], c == 0, c == 7)
                              for c in range(8)], [wpq, xTt], [pq[bnk]])
                    P.copy("act", qT[:, bnk * 4:(bnk + 1) * 4, :], pq[bnk][:], [pq[bnk]], [qT])
                for bnk in range(4):
                    P.mm([(pq[bnk][:, jj, :], qT[:, bnk * 4 + jj, :], ksT[:, (bnk * 4 + jj) % 2, :], True, True)
                          for jj in range(4)], [qT, ksT], [pq[bnk]])
                    P.copy("act", S[:, bnk * 4:(bnk + 1) * 4, :], pq[bnk][:], [pq[bnk]], [Su[bnk]])

            def chain(ti):
                S = Sb[ti % 2]
                Su = Sub[ti % 2]
                rtt = rt[ti % 2]
                top16_rows([(S[:, j, :], Su[j // 4], Swu[j], Vu[j], Iu[j]) for j in range(16)])
                Vv = V[:].rearrange("p (h two) r -> p h two r", two=2)
                Ifv = If[:].rearrange("p (h two) r -> p h two r", two=2)
                P.tt("pool", cand[:].rearrange("p h (a b) -> p h a b", a=16),
                     Vv[:, :, 0, :].unsqueeze(3).broadcast_to([128, 8, 16, 16]),
                     Vv[:, :, 1, :].unsqueeze(2).broadcast_to([128, 8, 16, 16]), ALU.add, Vu, [cand])
                P.copy("dve", If[:], I[:], Iu, [If])
                top16_rows([(cand[:, h, :], cand, Sw2u[h], scu[h], posu[h]) for h in range(8)])
                iota16 = cst[:, C_IOTA, 0:16].unsqueeze(1).unsqueeze(1).broadcast_to([128, 8, 16, 16])
                P.tt("dve", ex[:], scv[:], scv[:, :, 0:1].broadcast_to([128, 8, 16]), ALU.subtract, scu, [ex])
                P.act(ex[:], ex[:], AF.Exp, [ex], [ex])
                P.add("dve", lambda e: e.tensor_single_scalar(out=pa_[:], in_=pos[:], scalar=4,
                                                              op=ALU.logical_shift_right), posu, [pa_])
                P.add("dve", lambda e: e.tensor_single_scalar(out=pb_[:], in_=pos[:], scalar=15, op=ALU.bitwise_and),
                      posu, [pb_])
                P.copy("dve", af[:], pa_[:], [pa_], [af])
                P.copy("dve", bf[:], pb_[:], [pb_], [bf])
                P.tt("dve", oh[:], iota16, af[:].unsqueeze(3).broadcast_to([128, 8, 16, 16]), ALU.is_equal,
                     [cst, af], [oh])
                P.tt("dve", oh2[:], iota16, bf[:].unsqueeze(3).broadcast_to([128, 8, 16, 16]), ALU.is_equal,
                     [cst, bf], [oh2])
                P.tt("pool", oh[:], oh[:], Ifv[:, :, 0, :].unsqueeze(2).broadcast_to([128, 8, 16, 16]), ALU.mult,
                     [oh, If], [oh])
                P.tt("pool", oh2[:], oh2[:], Ifv[:, :, 1, :].unsqueeze(2).broadcast_to([128, 8, 16, 16]), ALU.mult,
                     [oh2, If], [oh2])
                P.add("dve", lambda e: e.reduce_sum(out=sm8[:], in_=ex[:], axis=AX.X), [ex], [sm8])
                P.recip(sm8[:], sm8[:], [sm8], [sm8])
                P.tt("dve", res[:, 2, :].rearrange("p (h r) -> p h r", h=8), ex[:],
                     sm8[:].unsqueeze(2).broadcast_to([128, 8, 16]), ALU.mult, [ex, sm8], [res])
                P.add("dve", lambda e: e.reduce_sum(out=res[:, 0, :].rearrange("p (h r) -> p h r", h=8),
                                                    in_=oh[:], axis=AX.X), [oh], [res])
                P.add("dve", lambda e: e.reduce_sum(out=res[:, 1, :].rearrange("p (h r) -> p h r", h=8),
                                                    in_=oh2[:], axis=AX.X), [oh2], [res])
                P.tr([(ptr[:, w, :], res[:, w, :], identf) for w in range(3)], [res, cst], [ptr])
                P.copy("act", rtt[:], ptr[:], [ptr], [rtt])
                P.dma(rt_d[ti].rearrange("p (w t) -> p w t", w=3), rtt[:], [rtt], [rt_d], f"o{ti%2}", eng="pool")

            front(0)
            for ti in range(NT):
                if ti + 1 < NT:
                    front(ti + 1)
                chain(ti)
            P.phase_end()

        with ExitStack() as es:
            GT = 3
            G = GT * 128
            TB = 8
            gfin = P.sb(es, "gfinP", [128, 1024], F32)
            WT = P.sb(es, "WT", [128, G, 128], BF16)
            xg = P.sb(es, "xg", [128, 8, G], BF16)
            x2g = [P.sb(es, f"x2g{i}", [128, 1024], F32) for i in range(GT)]
            rtg = [P.sb(es, f"rtg{i}", [128, 3, 128], F32) for i in range(GT)]
            Aoh = [P.sb(es, f"Aoh{i}", [128, TB, 128], BF16) for i in range(2)]
            Boh = [P.sb(es, f"Boh{i}", [128, TB, 128], BF16) for i in range(2)]
            iotab = P.sb(es, "iotab", [128, 128], BF16)
            ub = [P.sb(es, f"ub{i}", [128, 8, 512], BF16) for i in range(3)]
            vb = [P.sb(es, f"vbb{i}", [128, 4, 1024], BF16) for i in range(3)]
            hg = [P.sb(es, f"hg{i}", [128, G], F32) for i in range(2)]
            GA = [P.sb(es, f"GA{i}", [128, G], BF16) for i in range(2)]
            x3 = P.sb(es, "x3", [128, 1024], F32)
            sw = P.sb(es, "swQ", [128, 8], F32)
            yt = [P.sb(es, f"ytQ{i}", [128, 1024], F32) for i in range(2)]
            po = [[P.ps(es, f"po{t}{h}", [128, 512], F32) for h in range(2)] for t in range(GT)]
            ph = [P.ps(es, f"ph{i}", [128, 512], F32) for i in range(2)]
            uk = ["ld0", "ld1", "ld2"]
            vk = ["ws0", "ws1", "ld3"]
            P.dma(gfin[:], g_final.partition_broadcast(128), [], [gfin], "c")
            P.copy("dve", iotab[:], cst[:, C_IOTA, :], [cst], [iotab])
            yi = 0
            blk = 0
            for g0 in range(0, NT, GT):
                tiles = list(range(g0, min(NT, g0 + GT)))
                ng = len(tiles) * 128
                for tt, ti in enumerate(tiles):
                    P.dma(rtg[tt][:], rt_d[ti].rearrange("p (w t) -> p w t", w=3), [rt_d], [rtg[tt]], "misc0")
                for tt, ti in enumerate(tiles):
                    P.dma(x2g[tt][:], x2_d[ti * 128:(ti + 1) * 128, :], [x2_d], [x2g[tt]], "x0")
                    P.dma(xg[:, :, tt * 128:(tt + 1) * 128], xn2T_d[ti].rearrange("p (c t) -> p c t", c=8), [xn2T_d],
                          [xg], "st0")
                wi = 0
                for tt, ti in enumerate(tiles):
                    for sbk in range(128 // TB):
                        A = Aoh[sbk % 2]
                        B = Boh[sbk % 2]
                        t0 = sbk * TB
                        for k in range(TB):
                            t = t0 + k
                            P.ts("dve", A[:, k, :], iotab[:], rtg[tt][:, 0, t:t + 1], rtg[tt][:, 2, t:t + 1],
                                 ALU.is_equal, ALU.mult, [iotab, rtg[tt]], [A])
                            P.ts("dve", B[:, k, :], iotab[:], rtg[tt][:, 1, t:t + 1], None, ALU.is_equal, None,
                                 [iotab, rtg[tt]], [B])
                        for q4 in range(TB // 4):
                            pwb = ph[wi % 2]
                            wi += 1
                            P.mm([(pwb[:, k * 128:(k + 1) * 128], B[:, q4 * 4 + k, :], A[:, q4 * 4 + k, :], True, True)
                                  for k in range(4)], [A, B], [pwb])
                            tg = tt * 128 + t0 + q4 * 4
                            P.copy("act", WT[:, tg:tg + 4, :].rearrange("p t i -> p (t i)"), pwb[:], [pwb], [WT])
                bufs = {}

                def load_block(cb4):
                    nonlocal blk
                    ui = blk % 3
                    blk += 1
                    P.dma(ub[ui][:], u_bf[cb4], [u_bf], [ub[ui]], uk[ui])
                    P.dma(vb[ui][:], v_bf[cb4], [v_bf], [vb[ui]], vk[ui])
                    bufs[cb4] = ui

                def emit_H(c):
                    ui = bufs[c // 4]
                    cl = c % 4
                    phb = ph[c % 2]
                    P.mm([(phb[:, 0:ng], ub[ui][:, dc, cl * 128:(cl + 1) * 128], xg[:, dc, 0:ng], dc == 0, dc == 7)
                          for dc in range(8)], [ub[ui], xg], [phb])

                load_block(0)
                load_block(1)
                emit_H(0)
                for c in range(128):
                    if c % 4 == 0 and c // 4 + 2 < 32:
                        load_block(c // 4 + 2)
                    if c + 1 < 128:
                        emit_H(c + 1)
                    ui = bufs[c // 4]
                    cl = c % 4
                    phb = ph[c % 2]
                    hgb = hg[c % 2]
                    gab = GA[c % 2]
                    P.act(hgb[:, 0:ng], phb[:, 0:ng], AF.Gelu_apprx_tanh, [phb], [hgb])
                    P.tt("dve", gab[:, 0:ng], hgb[:, 0:ng], WT[:, 0:ng, c], ALU.mult,
                         [hgb, WT], [gab])
                    lst = []
                    for tt in range(len(tiles)):
                        for hh in range(2):
                            lst.append((po[tt][hh][:], gab[:, tt * 128:(tt + 1) * 128],
                                        vb[ui][:, cl, hh * 512:(hh + 1) * 512], c == 0, c == 127))
                    P.mm(lst, [gab, vb[ui]], [po[tt][hh] for tt in range(len(tiles)) for hh in range(2)])
                for tt, ti in enumerate(tiles):
                    yo = yt[yi % 2]
                    for hh in range(2):
                        P.tt("dve", x3[:, hh * 512:(hh + 1) * 512], po[tt][hh][:], x2g[tt][:, hh * 512:(hh + 1) * 512],
                             ALU.add, [po[tt][hh], x2g[tt]], [x3])
                    P.act(yo[:], x3[:], AF.Square, [x3], [yo, sw], accum=sw[:, 0:1])
                    P.act(sw[:, 1:2], sw[:, 0:1], AF.Sqrt, [sw], [sw], bias=EPS, scale=1.0 / 1024)
                    P.recip(sw[:, 1:2], sw[:, 1:2], [sw], [sw])
                    P.stt("dve", yo[:], x3[:], sw[:, 1:2], gfin[:], ALU.mult, ALU.mult, [x3, sw, gfin], [yo])
                    P.dma(y_o[ti * 128:(ti + 1) * 128, :], yo[:], [yo], [], f"sc{yi%2}", eng="pool")
                    yi += 1
            P.phase_end(final=True)
    return nc


def shard_inputs(inp, NTP=16):
    f = lambda a: np.ascontiguousarray(np.asarray(a, dtype=np.float32))
    xp, xs = f(inp["x_prompt"]), f(inp["x_sample"])
    consts = make_consts()
    u_expT = np.ascontiguousarray(f(inp["u_exp"])[0].T)
    shared = {
        "g_mix": f(inp["g_mix"])[0], "w_in": f(inp["w_in"])[0], "b_in": f(inp["b_in"])[0],
        "g_mlh": f(inp["g_mlh"])[0], "g_sgu": f(inp["g_sgu"])[0], "w_s": f(inp["w_s"])[0], "b_s": f(inp["b_s"])[0],
        "g_mem": f(inp["g_mem"])[0], "w_mk": f(inp["w_mk"])[0], "w_mv": f(inp["w_mv"])[0], "w_out": f(inp["w_out"])[0],
        "g_ffn": f(inp["g_ffn"])[0], "w_pq": f(inp["w_pq"])[0], "k_sub1": f(inp["k_sub1"])[0],
        "k_sub2": f(inp["k_sub2"])[0], "u_expT": u_expT, "v_exp": f(inp["v_exp"])[0], "g_final": f(inp["g_final"]),
        "consts": consts,
    }
    maps = []
    for c in range(8):
        b0, b1 = 2 * c, 2 * c + 2
        m = dict(shared)
        m["xall"] = np.ascontiguousarray(np.concatenate(
            [xp[b0:b1].reshape(-1, 1024), xs[b0:b1].reshape(-1, 1024)], axis=0))
        m["mem"] = np.ascontiguousarray(f(inp["mem_prompt"])[b0:b1].reshape(512, 1024))
        m["cmk"] = np.ascontiguousarray(f(inp["cache_mem_k"])[0, b0:b1].reshape(2, 256, 1024))
        m["cmv"] = np.ascontiguousarray(f(inp["cache_mem_v"])[0, b0:b1].reshape(2, 256, 1024))
        m["sC"] = np.ascontiguousarray(f(inp["state_mlstm_C"])[0, b0:b1])
        m["sn"] = np.ascontiguousarray(f(inp["state_mlstm_n"])[0, b0:b1])
        m["sm"] = np.ascontiguousarray(f(inp["state_mlstm_m"])[0, b0:b1])
        maps.append(m)
    return maps


def gather_outputs(results, NTP=16):
    S = NTP * 128
    cat = lambda k: np.concatenate([r[k] for r in results], axis=0)
    y = [r["y"] for r in results]
    y_prompt = np.concatenate([a[:2 * S].reshape(2, S, 1024) for a in y], axis=0)
    y_sample = np.concatenate([a[2 * S:].reshape(2, 64, 1024) for a in y], axis=0)
    return (y_prompt, y_sample, cat("Cp")[None], cat("np")[None], cat("mp")[None],
            cat("mk").reshape(16, 256, 4, 256)[None], cat("mv").reshape(16, 256, 4, 256)[None],
            cat("Cs")[None], cat("ns")[None], cat("ms")[None],
            np.concatenate([r["sguv"].reshape(2, 64, 1024) for r in results], axis=0)[None])


_NC_CACHE = {}


def kernel(**inputs):
    NTP = inputs["x_prompt"].shape[1] // 128
    peer = inputs.pop("_peer", True) if "_peer" in inputs else True
    key = (NTP, peer)
    if key not in _NC_CACHE:
        _NC_CACHE[key] = build(NTP, peer)
    nc = _NC_CACHE[key]
    maps = shard_inputs(inputs, NTP)
    res = run_bass_kernel_spmd(nc, maps, core_ids=list(range(8)))
    outs = gather_outputs(res.results, NTP)
    return tuple(np.ascontiguousarray(o, dtype=np.float32) for o in outs)
```

```python
import math
import numpy as np
from contextlib import ExitStack
import concourse.bass as bass
import concourse.mybir as mybir
from concourse.bass_utils import run_bass_kernel_spmd

F32 = mybir.dt.float32
BF16 = mybir.dt.bfloat16
U32 = mybir.dt.uint32
I32 = mybir.dt.int32
AF = mybir.ActivationFunctionType
ALU = mybir.AluOpType
AX = mybir.AxisListType
EPS = 1e-6
import os as _os
PIPE_B = _os.environ.get("PIPE_B", "1") == "1"
PIPE_C = _os.environ.get("PIPE_C", "1") == "1"


class Unit:
    __slots__ = ("name", "lw", "rd")

    def __init__(self, name):
        self.name = name
        self.lw = None
        self.rd = []


class T:
    def __init__(self, ap, name):
        self.ap = ap
        self.u = Unit(name)

    def __getitem__(self, k):
        return self.ap[k]


class Op:
    __slots__ = ("eng", "emit", "deps", "idx", "dma", "sem", "sigval", "needed", "dwait")


class Prog:
    ENGS = ("sp", "act", "dve", "pool", "pe")

    def __init__(self, nc, es, dma_keys):
        self.nc = nc
        self.es = es
        self.ops = []
        self.by_eng = {e: [] for e in self.ENGS}
        self.eng_sem = {e: es.enter_context(nc.semaphore("s_" + e)) for e in self.ENGS}
        self.dsem = {k: es.enter_context(nc.semaphore("d_" + k)) for k in dma_keys}
        self.cnt = {e: 0 for e in self.ENGS}
        self.dcnt = {k: 0 for k in dma_keys}
        self.waited = {e: {} for e in self.ENGS}
        self.flushed = 0

    def sb(self, es, name, shape, dtype):
        h = es.enter_context(self.nc.sbuf_tensor(name, list(shape), dtype))
        return T(h[tuple(slice(None) for _ in shape)], name)

    def ps(self, es, name, shape, dtype):
        h = es.enter_context(self.nc.psum_tensor(name, list(shape), dtype))
        return T(h[tuple(slice(None) for _ in shape)], name)

    def add(self, eng, emit, reads=(), writes=(), dma=None):
        op = Op()
        op.eng = eng
        op.emit = emit
        op.idx = len(self.ops)
        op.dma = dma
        op.needed = dma is not None
        op.sem = None
        op.sigval = None
        deps = set()
        for t in reads:
            u = t.u
            if u.lw is not None:
                deps.add(u.lw)
        for t in writes:
            u = t.u
            if u.lw is not None:
                deps.add(u.lw)
            deps.update(u.rd)
        for t in reads:
            t.u.rd.append(op.idx)
        for t in writes:
            t.u.lw = op.idx
            t.u.rd = []
        deps.discard(op.idx)
        op.deps = deps
        op.dwait = {}
        for d in deps:
            dk = self.ops[d].dma
            if dk is not None:
                op.dwait[dk] = self.dcnt[dk]
        if dma is not None:
            self.dcnt[dma] += 16
            op.sem = self.dsem[dma]
            op.sigval = self.dcnt[dma]
        self.ops.append(op)
        self.by_eng[eng].append(op)
        return op

    def dma(self, out, in_, reads, writes, key, eng="sp", **kw):
        return self.add(eng, lambda e: e.dma_start(out=out, in_=in_, **kw), reads, writes, dma=key)

    def tt(self, eng, out, in0, in1, op, reads, writes):
        return self.add(eng, lambda e: e.tensor_tensor(out=out, in0=in0, in1=in1, op=op), reads, writes)

    def ts(self, eng, out, in0, s1, s2, op0, op1, reads, writes, accum=None):
        if op1 is None:
            return self.add(eng, lambda e: e.tensor_scalar(out=out, in0=in0, scalar1=s1, scalar2=None, op0=op0),
                            reads, writes)
        if accum is not None:
            return self.add(eng, lambda e: e.tensor_scalar(out=out, in0=in0, scalar1=s1, scalar2=s2, op0=op0,
                                                          op1=op1, accum_out=accum), reads, writes)
        return self.add(eng, lambda e: e.tensor_scalar(out=out, in0=in0, scalar1=s1, scalar2=s2, op0=op0, op1=op1),
                        reads, writes)

    def stt(self, eng, out, in0, scalar, in1, op0, op1, reads, writes):
        return self.add(eng, lambda e: e.scalar_tensor_tensor(out=out, in0=in0, scalar=scalar, in1=in1, op0=op0,
                                                             op1=op1), reads, writes)

    def act(self, out, in_, func, reads, writes, bias=0.0, scale=1.0, accum=None):
        if accum is not None:
            return self.add("act", lambda e: e.activation(out=out, in_=in_, func=func, bias=bias, scale=scale,
                                                          accum_out=accum), reads, writes)
        return self.add("act", lambda e: e.activation(out=out, in_=in_, func=func, bias=bias, scale=scale),
                        reads, writes)

    def copy(self, eng, out, in_, reads, writes):
        if eng == "act":
            return self.add("act", lambda e: e.copy(out=out, in_=in_), reads, writes)
        return self.add(eng, lambda e: e.tensor_copy(out=out, in_=in_), reads, writes)

    def memset(self, eng, ap, val, writes):
        return self.add(eng, lambda e: e.memset(ap, val), (), writes)

    def recip(self, out, in_, reads, writes):
        return self.add("dve", lambda e: e.reciprocal(out=out, in_=in_), reads, writes)

    def mm(self, lst, reads, writes):
        def emit(e):
            ins = None
            for (o, l, r, st, sp) in lst:
                ins = e.matmul(o, lhsT=l, rhs=r, start=st, stop=sp)
            return ins
        return self.add("pe", emit, reads, writes)

    def tr(self, lst, reads, writes):
        def emit(e):
            ins = None
            for (o, i, idn) in lst:
                ins = e.transpose(out=o, in_=i, identity=idn)
            return ins
        return self.add("pe", emit, reads, writes)

    def barrier(self):
        last = set()
        seen_e = set()
        seen_k = set()
        for op in reversed(self.ops[self.flushed:]):
            if op.emit is None:
                continue
            if op.dma is not None:
                if op.dma not in seen_k:
                    seen_k.add(op.dma)
                    last.add(op.idx)
            elif op.eng not in seen_e:
                seen_e.add(op.eng)
                last.add(op.idx)
        for e in self.ENGS:
            op = self.add(e, None)
            op.deps = set(last)
            op.dwait = dict(self.dcnt)

    def phase_end(self, final=False):
        self.barrier()
        self.flush(final)

    def flush(self, final=False):
        nc = self.nc
        ops = self.ops
        start = self.flushed
        new = ops[start:]

        def skip(op, dop):
            return dop.eng == "pe" and op.eng == "pe" and dop.dma is None and op.dma is None

        for op in new:
            for d in op.deps:
                if d < start:
                    continue
                dop = ops[d]
                if skip(op, dop):
                    continue
                dop.needed = True
        for op in new:
            if op.emit is None:
                continue
            if op.dma is not None:
                pass
            elif op.needed:
                self.cnt[op.eng] += 1
                op.sem = self.eng_sem[op.eng]
                op.sigval = self.cnt[op.eng]
        self.flushed = len(ops)

        def run(engname, e):
            waited = self.waited[engname]
            for op in self.by_eng[engname]:
                want = {}
                for d in op.deps:
                    if d < start:
                        continue
                    dop = ops[d]
                    if skip(op, dop) or dop.sigval is None:
                        continue
                    k = id(dop.sem)
                    v = op.dwait[dop.dma] if dop.dma is not None else dop.sigval
                    if k not in want or want[k][1] < v:
                        want[k] = (dop.sem, v)
                for k, (s, v) in want.items():
                    if waited.get(k, 0) >= v:
                        continue
                    e.wait_ge(s, v)
                    waited[k] = v
                if op.emit is None:
                    continue
                ins = op.emit(e)
                if op.sigval is not None:
                    ins.then_inc(op.sem, 16 if op.dma is not None else 1)
            if final and engname == "sp":
                for k, s in self.dsem.items():
                    if self.dcnt[k] > 0:
                        e.wait_ge(s, self.dcnt[k])

        with nc.Block() as block:
            @block.sync
            def _(e):
                run("sp", e)

            @block.scalar
            def _(e):
                run("act", e)

            @block.vector
            def _(e):
                run("dve", e)

            @block.gpsimd
            def _(e):
                run("pool", e)

            @block.tensor
            def _(e):
                run("pe", e)
        self.by_eng = {e: [] for e in self.ENGS}


C_IDENT, C_TRIU, C_TRIUS, C_ONES, C_R0, C_R1, C_IOTA, C_HM0ROW, C_HM1ROW, C_MISC = range(10)
NCONST = 10


def make_consts():
    c = np.zeros((128, NCONST, 128), np.float32)
    s = np.arange(128)[:, None]
    t = np.arange(128)[None, :]
    c[:, C_IDENT] = (s == t)
    c[:, C_TRIU] = (s <= t)
    c[:, C_TRIUS] = (s <= t) & ((s // 64) == (t // 64))
    c[:, C_ONES] = 1.0
    c[:, C_R0] = (s < 64) * np.ones_like(t)
    c[:, C_R1] = (s >= 64) * np.ones_like(t)
    c[:, C_IOTA] = t * np.ones_like(s)
    c[:, C_HM0ROW] = (t < 64) * np.ones_like(s)
    c[:, C_HM1ROW] = (t >= 64) * np.ones_like(s)
    c[:, C_MISC, 0] = (np.arange(128) < 64)
    c[:, C_MISC, 1] = (np.arange(128) >= 64)
    return c.reshape(128, NCONST * 128)


Q0, K0, V0, IG0, FG0, OG0, SU0, SV0, XQ0, G00, G10, G20, DIN = 0, 512, 1024, 2048, 2052, 2056, 3080, 4104, 5128, 6152, 7176, 8200, 9224

DMA_KEYS = ["c", "ws0", "ws1", "x0", "x1", "st0", "st1", "o0", "o1", "sc0", "sc1", "ld0", "ld1", "ld2", "ld3",
            "misc0", "misc1", "oC", "pst0", "pst1", "p3"]


def build(NTP=16, peer=True):
    NT = 2 * NTP + 1
    NTOK = NT * 128
    nc = bass.Bass("TRN2", target_bir_lowering=False)

    def din(name, shape, dt=F32):
        return nc.dram_tensor(name, list(shape), dt, kind="ExternalInput").ap()

    def dout(name, shape, dt=F32):
        return nc.dram_tensor(name, list(shape), dt, kind="ExternalOutput").ap()

    def dscr(name, shape, dt=F32):
        return nc.dram_tensor(name, list(shape), dt, kind="Internal").ap()

    xall = din("xall", [NTOK, 1024])
    mem = din("mem", [512, 1024])
    cmk = din("cmk", [2, 256, 1024])
    cmv = din("cmv", [2, 256, 1024])
    sC = din("sC", [2, 4, 128, 256])
    sn = din("sn", [2, 4, 128])
    sm = din("sm", [2, 4])
    g_mix = din("g_mix", [1024])
    w_in = din("w_in", [1024, DIN])
    b_in = din("b_in", [DIN])
    g_mlh = din("g_mlh", [1024])
    g_sgu = din("g_sgu", [1024])
    w_s = din("w_s", [4, 128, 128])
    b_s = din("b_s", [4, 128])
    g_mem = din("g_mem", [1024])
    w_mk = din("w_mk", [1024, 1024])
    w_mv = din("w_mv", [1024, 1024])
    w_out = din("w_out", [1024, 1024])
    g_ffn = din("g_ffn", [1024])
    w_pq = din("w_pq", [1024, 2048])
    k_sub1 = din("k_sub1", [128, 128])
    k_sub2 = din("k_sub2", [128, 128])
    u_expT = din("u_expT", [1024, 16384])
    v_exp = din("v_exp", [16384, 1024])
    g_final = din("g_final", [1024])
    consts_d = din("consts", [128, NCONST * 128])

    y_o = dout("y", [NTOK, 1024])
    Cp_o = dout("Cp", [2, 4, 128, 256])
    np_o = dout("np", [2, 4, 128])
    mp_o = dout("mp", [2, 4])
    mk_o = dout("mk", [2, 256, 1024])
    mv_o = dout("mv", [2, 256, 1024])
    Cs_o = dout("Cs", [2, 4, 128, 256])
    ns_o = dout("ns", [2, 4, 128])
    ms_o = dout("ms", [2, 4])
    sguv_o = dout("sguv", [128, 1024])

    xnT_d = T(dscr("xnT_d", [NT, 128, 1024], BF16), "xnT_d")
    ma_d = T(dscr("ma_d", [NTOK, 1024]), "ma_d")
    x2_d = T(dscr("x2_d", [NTOK, 1024]), "x2_d")
    u_bf = T(dscr("u_bf", [32, 128, 8, 512], BF16), "u_bf")
    v_bf = T(dscr("v_bf", [32, 128, 4, 1024], BF16), "v_bf")
    mk_u = T(mk_o, "mk_o")
    mv_u = T(mv_o, "mv_o")

    with ExitStack() as es0:
        P = Prog(nc, es0, DMA_KEYS)
        cst = P.sb(es0, "cst", [128, NCONST, 128], F32)
        identb = P.sb(es0, "identb", [128, 128], BF16)
        P.dma(cst[:].rearrange("p a b -> p (a b)"), consts_d, [], [cst], "misc1")
        P.copy("dve", identb[:], cst[:, C_IDENT, :], [cst], [identb])
        identf = cst[:, C_IDENT, :]

        def rmsnorm_T(es_unused, x_ap, x_t, junk, ss, rstd, xn, pT, xnT, eng_mul="dve"):
            P.act(junk[:], x_ap, AF.Square, [x_t], [junk, ss], accum=ss[:])
            P.act(rstd[:], ss[:], AF.Sqrt, [ss], [rstd], bias=EPS, scale=1.0 / 1024)
            P.recip(rstd[:], rstd[:], [rstd], [rstd])
            P.ts(eng_mul, xn[:], x_ap, rstd[:, 0:1], None, ALU.mult, None, [x_t, rstd], [xn])
            P.tr([(pT[:, c, :], xn[:, c * 128:(c + 1) * 128], identb[:]) for c in range(8)], [xn, identb], [pT])
            P.copy("act", xnT[:], pT[:], [pT], [xnT])

        def load_w(es, wt, w_dram, col_ranges, gvec, stg, nrow_chunks=8):
            i = 0
            for c in range(nrow_chunks):
                o = 0
                for (a, b) in col_ranges:
                    p = a
                    while p < b:
                        n = min(1024, b - p)
                        s = stg[i % 2]
                        P.dma(s[:, 0:n], w_dram[c * 128:(c + 1) * 128, p:p + n], [], [s], f"ws{i%2}")
                        if i % 2:
                            P.act(wt[:, c, o:o + n], s[:, 0:n], AF.Copy, [s, gvec], [wt], scale=gvec[:, c:c + 1])
                        else:
                            P.ts("dve", wt[:, c, o:o + n], s[:, 0:n], gvec[:, c:c + 1], None, ALU.mult, None,
                                 [s, gvec], [wt])
                        o += n
                        p += n
                        i += 1


        class P0:
            def __init__(self):
                self.next_piece = 0
                self.pending = []
                self.cs = self.cb = self.lk = None
                self.sk = ["sc0", "sc1", "p3"]

            def bind(self, cs, cb, lk):
                assert not self.pending
                self.cs, self.cb, self.lk = cs, cb, lk

            def load_some(self, n=3):
                for i in range(n):
                    if self.next_piece >= 128:
                        return
                    pc = self.next_piece
                    self.next_piece += 1
                    c_ = self.cs[i]
                    if pc < 64:
                        r0, c0 = (pc // 8) * 128, (pc % 8) * 2048
                        P.dma(c_[:], u_expT[r0:r0 + 128, c0:c0 + 2048], [], [c_], self.lk[i])
                    else:
                        q = pc - 64
                        src = v_exp[q * 256:(q + 1) * 256, :].rearrange("(a p) d -> p a d", a=2)
                        P.dma(c_[:].rearrange("p (a d) -> p a d", a=2), src, [], [c_], self.lk[i])
                    self.pending.append((pc, i))

            def cast_some(self):
                for (pc, i) in self.pending:
                    c_, b_ = self.cs[i], self.cb[i]
                    P.copy("act", b_[:], c_[:], [c_], [b_])
                    if pc < 64:
                        b0 = (pc % 8) * 4
                        P.dma(u_bf[b0:b0 + 4, :, pc // 8, :].rearrange("b p e -> p b e"),
                              b_[:].rearrange("p (b e) -> p b e", b=4), [b_], [u_bf], self.sk[i], eng="pool")
                    else:
                        q = pc - 64
                        dstv = v_bf[q // 2, :, (q % 2) * 2:(q % 2) * 2 + 2, :]
                        P.dma(dstv, b_[:].rearrange("p (a d) -> p a d", a=2), [b_], [v_bf], self.sk[i], eng="pool")
                self.pending = []

            def step(self):
                self.cast_some()
                self.load_some()

        p0 = P0() if peer else None

        with ExitStack() as es:
            gm = P.sb(es, "gm", [128, 8], F32)
            stg = [P.sb(es, f"stg{i}", [128, 1024], F32) for i in range(2)]
            wk = P.sb(es, "wk", [128, 8, 1024], BF16)
            wv = P.sb(es, "wv", [128, 8, 1024], BF16)
            xt = [P.sb(es, f"xt{i}", [128, 1024], F32) for i in range(2)]
            junk = P.sb(es, "junk", [128, 1024], F32)
            ss = P.sb(es, "ss", [128, 1], F32)
            rstd = P.sb(es, "rstd", [128, 1], F32)
            xn = P.sb(es, "xn", [128, 1024], BF16)
            xnT = P.sb(es, "xnT", [128, 8, 128], BF16)
            ot = [P.sb(es, f"ot{i}", [128, 1024], F32) for i in range(2)]
            pT = P.ps(es, "pT", [128, 8, 128], BF16)
            pm = [P.ps(es, f"pm{i}", [128, 512], F32) for i in range(2)]
            P.dma(gm[:], g_mem.rearrange("(c p) -> p c", p=128), [], [gm], "c", allow_slow_non_contiguous=True)
            load_w(es, wk, w_mk, [(0, 1024)], gm, stg)
            load_w(es, wv, w_mv, [(0, 1024)], gm, stg)
            oi = 0
            for t in range(4):
                x = xt[t % 2]
                P.dma(x[:], mem[t * 128:(t + 1) * 128, :], [], [x], f"x{t%2}")
                rmsnorm_T(es, x[:], x, junk, ss, rstd, xn, pT, xnT)
                for (wt, o_dram, o_u) in ((wk, mk_o, mk_u), (wv, mv_o, mv_u)):
                    o = ot[oi % 2]
                    for h in range(2):
                        P.mm([(pm[h][:], xnT[:, c, :], wt[:, c, h * 512:(h + 1) * 512], c == 0, c == 7)
                              for c in range(8)], [xnT, wt], [pm[h]])
                        P.copy("dve" if h == 0 else "act", o[:, h * 512:(h + 1) * 512], pm[h][:], [pm[h]], [o])
                    P.dma(o_dram[t // 2, (t % 2) * 128:(t % 2 + 1) * 128, :], o[:], [o], [o_u], f"o{oi%2}", eng="pool")
                    oi += 1
            P.phase_end()

        with ExitStack() as es:
            NA = 4104
            gm = P.sb(es, "gmA", [128, 8], F32)
            stg = [P.sb(es, f"stgA{i}", [128, 1024], F32) for i in range(2)]
            wA = P.sb(es, "wA", [128, 8, NA], BF16)
            oQ, oK, oV, oIG, oOG, oG0 = 0, 512, 1024, 2048, 2056, 3080
            bq = P.sb(es, "bq", [128, 8], F32)
            btok = P.sb(es, "btokA", [128, NA], F32)
            gmlh = P.sb(es, "gmlh", [128, 1024], F32)
            xt = [P.sb(es, f"xA{i}", [128, 1024], F32) for i in range(2)]
            junk = P.sb(es, "junkA", [128, 1024], F32)
            ss = P.sb(es, "ssA", [128, 1], F32)
            rstd = P.sb(es, "rstdA", [128, 1], F32)
            xn = P.sb(es, "xnA", [128, 1024], BF16)
            xnT = [P.sb(es, f"xnTA{i}", [128, 8, 128], BF16) for i in range(2)]
            qT = P.sb(es, "qT", [128, 4, 128], BF16)
            kT = P.sb(es, "kT", [128, 4, 128], BF16)
            qTm = [P.sb(es, f"qTm{i}", [128, 4, 128], BF16) for i in range(2)]
            ktok = P.sb(es, "ktok", [128, 512], F32)
            kts = [P.sb(es, f"kts{i}", [128, 512], BF16) for i in range(2)]
            vaug = P.sb(es, "vaug", [128, 4, 257], BF16)
            gsb = P.sb(es, "gsb", [128, 8], F32)
            gw = P.sb(es, "gw", [128, 40], F32)
            gateA = P.sb(es, "gateA", [128, 1024], F32)
            g0s = P.sb(es, "g0s", [128, 1024], F32)
            mat = [P.sb(es, f"mat{i}", [128, 1024], F32) for i in range(2)]
            Cf = [P.sb(es, f"Cf{i}", [128, 4, 257], F32) for i in range(2)]
            Cb = [P.sb(es, f"Cb{i}", [128, 4, 257], BF16) for i in range(2)]
            Cout = P.sb(es, "Cout", [128, 4, 257], F32)
            mst = [P.sb(es, f"mst{i}", [4, 1], F32) for i in range(2)]
            mw = P.sb(es, "mw", [4, 8], F32)
            mrow = P.sb(es, "mrow", [4, 128], F32)
            bc4 = P.sb(es, "bc4", [128, 4], F32)
            pT = P.ps(es, "pTA", [128, 8, 128], BF16)
            pf1 = P.ps(es, "pfA0", [128, 4, 128], F32)
            pt1 = P.ps(es, "ptA0", [128, 512], F32)
            pf1v = T(pf1[:].rearrange("p a b -> p (a b)"), "pf1v")
            pf1v.u = pf1.u
            pt1v = T(pt1[:].rearrange("p (a b) -> p a b", a=4), "pt1v")
            pt1v.u = pt1.u
            pf = [pf1, pt1v]
            pt = [pf1v, pt1]
            pS = P.ps(es, "pS", [128, 512], F32)
            X = [P.ps(es, f"XA{h}", [128, 512], F32) for h in range(4)]
            ATh = [P.sb(es, f"ATh{h}", [128, 128], BF16) for h in range(4)]
            hwh = [P.sb(es, f"hwh{h}", [128, 4], F32) for h in range(4)]
            Cfu = [[T(Cf[si][:, h, :], f"Cfu{si}{h}") for h in range(4)] for si in range(2)]
            Cbu = [[T(Cb[si][:, h, :], f"Cbu{si}{h}") for h in range(4)] for si in range(2)]
            junkh = [T(junk[:, h * 256:(h + 1) * 256], f"junkh{h}") for h in range(4)]

            if peer:
                csA = [P.sb(es, f"cs{i}", [128, 2048], F32) for i in range(3)]
                cbA = [P.sb(es, f"cb{i}", [128, 2048], BF16) for i in range(3)]
                p0.bind(csA, cbA, ["ld0", "ld1", "ld2"])
            P.dma(gm[:], g_mix.rearrange("(c p) -> p c", p=128), [], [gm], "c", allow_slow_non_contiguous=True)
            colsA = [(Q0, OG0 + 1024), (G00, G00 + 1024)]
            P.dma(btok[:, 0:3080], b_in[0:3080].partition_broadcast(128), [], [btok], "c")
            P.dma(btok[:, 3080:4104], b_in[G00:G00 + 1024].partition_broadcast(128), [], [btok], "c")
            P.dma(bq[:], b_in[0:1024].rearrange("(c p) -> p c", p=128), [], [bq], "c", allow_slow_non_contiguous=True)
            P.dma(gmlh[:], g_mlh.partition_broadcast(128), [], [gmlh], "c")
            load_w(es, wA, w_in, colsA, gm, stg)
            if p0 is not None:
                p0.load_some()
            P.memset("dve", vaug[:, :, 256:257], 1.0, [vaug])

            def bcast4(src41, dst):
                P.ts("dve", mrow[:], cst[0:4, C_ONES, :], src41, None, ALU.mult, None, [cst, mw, mst[0], mst[1]],
                     [mrow])
                P.mm([(pS[:, 300:304], mrow[:], cst[0:4, C_IDENT, 0:4], True, True)], [mrow, cst], [pS])
                P.copy("dve", dst, pS[:, 300:304], [pS], [bc4])

            def init_state(si, sample_b=None):
                if sample_b is None:
                    P.memset("dve", Cf[si][:], 0.0, Cfu[si])
                    P.memset("pool", Cb[si][:], 0.0, Cbu[si])
                    P.memset("dve", mst[si][:], 0.0, [mst[si]])
                else:
                    b = sample_b
                    P.dma(Cf[si][:, :, 0:256], sC[b].rearrange("h d v -> d h v"), [], Cfu[si], f"misc{si}")
                    P.dma(Cf[si][:, :, 256:257], sn[b].rearrange("h (d o) -> d h o", o=1), [], Cfu[si], f"misc{si}",
                          allow_slow_non_contiguous=True)
                    P.dma(mst[si][:], sm[b].rearrange("(h o) -> h o", o=1), [], [mst[si]], f"misc{si}",
                          allow_slow_non_contiguous=True)
                    P.act(mw[:, 0:1], mst[si][:], AF.Exp, [mst[si]], [mw])
                    bcast4(mw[:, 0:1], bc4[:])
                    P.tt("dve", Cf[si][:], Cf[si][:], bc4[:].unsqueeze(2).broadcast_to([128, 4, 257]), ALU.mult,
                         Cfu[si] + [bc4], Cfu[si])
                    P.copy("act", Cb[si][:], Cf[si][:], Cfu[si], Cbu[si])

            def final_state(si, C_dram, n_dram, m_dram, b):
                P.act(mw[:, 0:1], mst[si][:], AF.Exp, [mst[si]], [mw], scale=-1.0)
                bcast4(mw[:, 0:1], bc4[:])
                P.tt("dve", Cout[:], Cf[si][:], bc4[:].unsqueeze(2).broadcast_to([128, 4, 257]), ALU.mult,
                     Cfu[si] + [bc4], [Cout])
                P.dma(C_dram[b].rearrange("h d v -> d h v"), Cout[:, :, 0:256], [Cout], [], "oC", eng="pool")
                P.dma(n_dram[b].rearrange("h (d o) -> d h o", o=1), Cout[:, :, 256:257], [Cout], [], "oC", eng="pool",
                      allow_slow_non_contiguous=True)
                P.dma(m_dram[b].rearrange("(h o) -> h o", o=1), mst[si][:], [mst[si]], [], "oC", eng="pool",
                      allow_slow_non_contiguous=True)

            LNK = math.log(128 ** -0.5)
            mahs = {}
            for ti in range(NT):
                sample = (ti == NT - 1)
                x = xt[ti % 2]
                xT = xnT[ti % 2]
                ma = mat[ti % 2]
                if sample:
                    init_state(0, 0)
                    init_state(1, 1)
                    segs = [(0, C_R0, 0, 64), (1, C_R1, 64, 128)]
                elif ti % NTP == 0:
                    init_state(0)
                    segs = [(0, C_ONES, 0, 128)]
                else:
                    segs = [(0, C_ONES, 0, 128)]
                mask = cst[:, C_TRIUS if sample else C_TRIU, :]
                if ti == 0:
                    P.dma(x[:], xall[0:128, :], [], [x], "x0")
                if ti + 1 < NT:
                    xnx = xt[(ti + 1) % 2]
                    P.dma(xnx[:], xall[(ti + 1) * 128:(ti + 2) * 128, :], [], [xnx], f"x{(ti+1)%2}")
                rmsnorm_T(es, x[:], x, junk, ss, rstd, xn, pT, xT)
                P.dma(xnT_d[ti].rearrange("p (c t) -> p c t", c=8), xT[:], [xT], [xnT_d], f"pst{ti%2}", eng="pool")
                for j, (dst, c0, wo) in enumerate(((qT, 0, oQ), (kT, 4, oK))):
                    pp = pf[j]
                    for h in range(4):
                        P.mm([(pp[:, h, :], wA[:, c, wo + h * 128:wo + (h + 1) * 128], xT[:, c, :], c == 0, c == 7)
                              for c in range(8)], [wA, xT], [pp])
                    P.tt("dve", dst[:], pp[:], bq[:, c0:c0 + 4].unsqueeze(2).broadcast_to([128, 4, 128]), ALU.add,
                         [pp, bq], [dst])
                pi = [0]

                def tok_block(wo, n, dst_ap, dst_t, eng="dve"):
                    pp = pt[pi[0] % 2]
                    pi[0] += 1
                    P.mm([(pp[:, 0:n], xT[:, c, :], wA[:, c, wo:wo + n], c == 0, c == 7) for c in range(8)],
                         [xT, wA], [pp])
                    P.tt(eng, dst_ap, pp[:, 0:n], btok[:, wo:wo + n], ALU.add, [pp, btok], [dst_t])

                tok_block(oIG, 8, gsb[:], gsb)
                tok_block(oK, 512, ktok[:], ktok)
                for hh in range(2):
                    pp = pt[pi[0] % 2]
                    pi[0] += 1
                    wo = oV + hh * 512
                    P.mm([(pp[:], xT[:, c, :], wA[:, c, wo:wo + 512], c == 0, c == 7) for c in range(8)],
                         [xT, wA], [pp])
                    P.tt("dve", vaug[:, 2 * hh:2 * hh + 2, 0:256], pp[:].rearrange("p (h v) -> p h v", h=2),
                         btok[:, wo:wo + 512].rearrange("p (h v) -> p h v", h=2), ALU.add, [pp, btok], [vaug])
                for hh in range(2):
                    tok_block(oOG + hh * 512, 512, gateA[:, hh * 512:(hh + 1) * 512], gateA)
                for hh in range(2):
                    tok_block(oG0 + hh * 512, 512, g0s[:, hh * 512:(hh + 1) * 512], g0s)
                P.act(gateA[:], gateA[:], AF.Sigmoid, [gateA], [gateA])
                P.act(g0s[:], g0s[:], AF.Sigmoid, [g0s], [g0s])
                P.tt("dve", gateA[:], gateA[:], g0s[:], ALU.mult, [gateA, g0s], [gateA])
                P.tt("dve", gateA[:], gateA[:], gmlh[:], ALU.mult, [gateA, gmlh], [gateA])
                P.act(gw[:, 0:4], gsb[:, 4:8], AF.Exp, [gsb], [gw], scale=-1.0)
                P.act(gw[:, 0:4], gw[:, 0:4], AF.Ln, [gw], [gw], bias=1.0)
                lst = [(pS[:, 256:260], mask, gw[:, 0:4], True, True)]
                for k, (si, rc, lo, hi) in enumerate(segs):
                    lst.append((pS[:, 260 + 4 * k:264 + 4 * k], cst[:, rc, :], gw[:, 0:4], True, True))
                P.mm(lst, [cst, gw], [pS])
                nsg = 4 * len(segs)
                P.copy("dve", gw[:, 4:8 + nsg], pS[:, 256:260 + nsg], [pS], [gw])
                P.tt("dve", gw[:, 16:20], gsb[:, 0:4], gw[:, 4:8], ALU.add, [gsb, gw], [gw])
                P.act(gw[:, 20:24], gw[:, 16:20], AF.Exp, [gw], [gw], bias=LNK)
                P.act(gw[:, 24:28], gw[:, 4:8], AF.Exp, [gw], [gw])
                P.act(gw[:, 28:28 + nsg], gw[:, 8:8 + nsg], AF.Exp, [gw], [gw], scale=-1.0)
                P.tr([(pS[0:4, 384:512], gw[:, 16:20], identf)], [gw, cst], [pS])
                P.copy("dve", mrow[:], pS[0:4, 384:512], [pS], [mrow])
                for k, (si, rc, lo, hi) in enumerate(segs):
                    P.add("dve", lambda e, lo=lo, hi=hi: e.reduce_max(out=mw[:, 1:2], in_=mrow[:, lo:hi], axis=AX.X),
                          [mrow], [mw])
                    P.tt("dve", mw[:, 1:2], mw[:, 1:2], mst[si][:], ALU.max, [mw, mst[si]], [mw])
                    P.tr([(pS[0:4, 384:512], gw[:, 8 + 4 * k:12 + 4 * k], identf)], [gw, cst], [pS])
                    P.tt("dve", mst[si][:], mw[:, 1:2], pS[0:4, 384 + lo:385 + lo], ALU.subtract, [mw, pS], [mst[si]])
                for k, (si, rc, lo, hi) in enumerate(segs):
                    for h in range(4):
                        P.ts("dve", kts[k][:, h * 128:(h + 1) * 128], ktok[:, h * 128:(h + 1) * 128],
                             gw[:, 20 + h:21 + h], None, ALU.mult, None, [ktok, gw], [kts[k]])
                    if sample:
                        P.ts("pool", kts[k][:], kts[k][:], cst[:, C_MISC, k:k + 1], None, ALU.mult, None,
                             [kts[k], cst], [kts[k]])
                        P.tt("pool", qTm[k][:], qT[:], cst[:, C_HM0ROW + k, :].unsqueeze(1).broadcast_to([128, 4, 128]),
                             ALU.mult, [qT, cst], [qTm[k]])
                mah = [T(ma[:, h * 256:(h + 1) * 256], f"mah{ti%2}{h}") for h in range(4)] if ti < 2 else mahs[ti % 2]
                mahs[ti % 2] = mah
                H4 = range(4)
                for h in H4:
                    P.mm([(X[h][:, 0:128], kT[:, h, :], qT[:, h, :], True, True)], [kT, qT], [X[h]])
                for h in H4:
                    P.stt("dve", ATh[h][:], X[h][:, 0:128], gw[:, 20 + h:21 + h], mask, ALU.mult, ALU.mult,
                          [X[h], gw, cst], [ATh[h]])
                for h in H4:
                    lst = [(X[h][:, 128:385], ATh[h][:], vaug[:, h, :], True, False)]
                    rd = [ATh[h], vaug, qT]
                    for k, (si, rc, lo, hi) in enumerate(segs):
                        qsrc = qTm[k] if sample else qT
                        lst.append((X[h][:, 128:385], qsrc[:, h, :], Cb[si][:, h, :], False, k == len(segs) - 1))
                        rd += [qsrc, Cbu[si][h]]
                    P.mm(lst, rd, [X[h]])
                for h in H4:
                    P.act(hwh[h][:, 0:1], X[h][:, 384:385], AF.Abs, [X[h]], [hwh[h]])
                for h in H4:
                    P.tt("dve", hwh[h][:, 0:1], hwh[h][:, 0:1], gw[:, 24 + h:25 + h], ALU.max, [hwh[h], gw], [hwh[h]])
                for h in H4:
                    P.recip(hwh[h][:, 0:1], hwh[h][:, 0:1], [hwh[h]], [hwh[h]])
                for h in H4:
                    P.act(junkh[h][:], X[h][:, 128:384], AF.Square, [X[h], hwh[h]], [junkh[h], hwh[h]],
                          scale=hwh[h][:, 0:1], accum=hwh[h][:, 1:2])
                for h in H4:
                    P.act(hwh[h][:, 2:3], hwh[h][:, 1:2], AF.Sqrt, [hwh[h]], [hwh[h]], bias=EPS, scale=1.0 / 256)
                for h in H4:
                    P.recip(hwh[h][:, 2:3], hwh[h][:, 2:3], [hwh[h]], [hwh[h]])
                for h in H4:
                    P.tt("dve", hwh[h][:, 3:4], hwh[h][:, 2:3], hwh[h][:, 0:1], ALU.mult, [hwh[h]], [hwh[h]])
                for h in H4:
                    P.stt("dve", ma[:, h * 256:(h + 1) * 256], X[h][:, 128:384], hwh[h][:, 3:4],
                          gateA[:, h * 256:(h + 1) * 256], ALU.mult, ALU.mult, [X[h], hwh[h], gateA], [mah[h]])
                for k, (si, rc, lo, hi) in enumerate(segs):
                    for h in H4:
                        P.mm([(X[h][:, 128:385], kts[k][:, h * 128:(h + 1) * 128], vaug[:, h, :], True, True)],
                             [kts[k], vaug], [X[h]])
                    for h in H4:
                        eb = gw[:, 28 + 4 * k + h:29 + 4 * k + h]
                        P.ts("dve", Cf[si][:, h, :], Cf[si][:, h, :], eb, None, ALU.mult, None, [Cfu[si][h], gw],
                             [Cfu[si][h]])
                    for h in H4:
                        eb = gw[:, 28 + 4 * k + h:29 + 4 * k + h]
                        P.stt("dve", Cf[si][:, h, :], X[h][:, 128:385], eb, Cf[si][:, h, :], ALU.mult, ALU.add,
                              [X[h], gw, Cfu[si][h]], [Cfu[si][h]])
                    for h in H4:
                        P.copy("act", Cb[si][:, h, :], Cf[si][:, h, :], [Cfu[si][h]], [Cbu[si][h]])
                P.dma(ma_d[ti * 128:(ti + 1) * 128, :], ma[:], mah, [ma_d], f"o{ti%2}", eng="pool")
                if p0 is not None:
                    p0.step()
                if sample:
                    final_state(0, Cs_o, ns_o, ms_o, 0)
                    final_state(1, Cs_o, ns_o, ms_o, 1)
                elif ti % NTP == NTP - 1:
                    final_state(0, Cp_o, np_o, mp_o, ti // NTP)
            if p0 is not None:
                p0.cast_some()
            P.phase_end()

        with ExitStack() as es:
            NB = 3072
            oSU, oSV, oG1 = 0, 1024, 2048
            gm = P.sb(es, "gmB", [128, 8], F32)
            stg = [P.sb(es, f"stgB{i}", [128, 1024], F32) for i in range(2)]
            wB = P.sb(es, "wB", [128, 8, NB], BF16)
            btok = P.sb(es, "btokB", [128, NB], F32)
            gsgu = P.sb(es, "gsgu", [128, 1024], F32)
            wsT = [P.sb(es, f"wsT{i}", [128, 4, 128], BF16) for i in range(2)]
            wsl = [P.sb(es, f"wsl{i}", [128, 4, 128], F32) for i in range(2)]
            ltri = P.sb(es, "ltri", [128, 2, 128], F32)
            bs = [P.sb(es, f"bs{i}", [128, 4], F32) for i in range(2)]
            xT = [P.sb(es, f"xTB{i}", [128, 8, 128], BF16) for i in range(3)]
            mat = [P.sb(es, f"maB{i}", [128, 1024], F32) for i in range(3)]
            kxT = ["st0", "st1", "x0"]
            kma = ["ld2", "ld3", "x1"]
            gv = P.sb(es, "gv", [128, 1024], F32)
            g1 = P.sb(es, "g1", [128, 1024], F32)
            vn = [P.sb(es, f"vn{i}", [128, 1024], F32) for i in range(2)]
            vnb2 = [P.sb(es, f"vnb{i}", [128, 1024], BF16) for i in range(2)]
            ub2 = [P.sb(es, f"ub2{i}", [128, 1024], F32) for i in range(2)]
            junk = P.sb(es, "junkB", [128, 1024], F32)
            junk2 = P.sb(es, "junkB2", [128, 1024], F32)
            sw = P.sb(es, "sw", [128, 16], F32)
            mhalf = P.sb(es, "mhalfB", [128, 1], F32)
            P.memset("dve", mhalf[:], -0.5, [mhalf])
            pt = [P.ps(es, f"ptB{i}", [128, 512], F32) for i in range(2)]
            pa = [P.ps(es, f"paB{i}", [128, 512], F32) for i in range(2)]

            if p0 is not None:
                csB = [P.sb(es, f"csB{i}", [128, 2048], F32) for i in range(3)]
                cbB = [P.sb(es, f"cbB{i}", [128, 2048], BF16) for i in range(3)]
                p0.bind(csB, cbB, ["ld0", "ld1", "misc1"])
            P.dma(gm[:], g_mix.rearrange("(c p) -> p c", p=128), [], [gm], "c", allow_slow_non_contiguous=True)
            P.dma(btok[:, 0:2048], b_in[SU0:XQ0].partition_broadcast(128), [], [btok], "c")
            P.dma(btok[:, 2048:3072], b_in[G10:G20].partition_broadcast(128), [], [btok], "c")
            P.dma(gsgu[:], g_sgu.partition_broadcast(128), [], [gsgu], "c")
            P.dma(wsl[0][:], w_s.rearrange("g t s -> t g s"), [], [wsl[0]], "c")
            P.dma(bs[0][:], b_s.rearrange("g t -> t g"), [], [bs[0]], "c", allow_slow_non_contiguous=True)
            P.dma(bs[1][0:64, :], b_s[:, 0:64].rearrange("g t -> t g"), [], [bs[1]], "c", allow_slow_non_contiguous=True)
            P.dma(bs[1][64:128, :], b_s[:, 0:64].rearrange("g t -> t g"), [], [bs[1]], "c",
                  allow_slow_non_contiguous=True)
            P.memset("dve", wsl[1][:], 0.0, [wsl[1]])
            P.dma(wsl[1][0:64, :, 0:64], w_s[:, 0:64, 0:64].rearrange("g t s -> t g s"), [], [wsl[1]], "misc0")
            P.dma(wsl[1][64:128, :, 64:128], w_s[:, 0:64, 0:64].rearrange("g t s -> t g s"), [], [wsl[1]], "misc0")
            load_w(es, wB, w_in, [(SU0, XQ0), (G10, G20)], gm, stg)
            P.tr([(pt[0][:, 0:128], cst[:, C_TRIU, :], identf), (pt[0][:, 128:256], cst[:, C_TRIUS, :], identf)],
                 [cst], [pt[0]])
            P.copy("dve", ltri[:].rearrange("p a b -> p (a b)"), pt[0][:, 0:256], [pt[0]], [ltri])
            for i in range(2):
                P.tt("dve", wsl[i][:], wsl[i][:], ltri[:, i, :].unsqueeze(1).broadcast_to([128, 4, 128]), ALU.mult,
                     [wsl[i], ltri], [wsl[i]])
                P.tr([(pa[i][:, g * 128:(g + 1) * 128], wsl[i][:, g, :], identf) for g in range(4)], [wsl[i], cst],
                     [pa[i]])
                P.copy("dve", wsT[i][:].rearrange("p g t -> p (g t)"), pa[i][:], [pa[i]], [wsT[i]])

            def frontB(ti):
                sample = (ti == NT - 1)
                xTt = xT[ti % 3]
                ma = mat[ti % 3]
                vnt = vn[ti % 2]
                ut = ub2[ti % 2]
                vb_ = vnb2[ti % 2]
                P.dma(xTt[:], xnT_d[ti].rearrange("p (c t) -> p c t", c=8), [xnT_d], [xTt], kxT[ti % 3])
                P.dma(ma[:], ma_d[ti * 128:(ti + 1) * 128, :], [ma_d], [ma], kma[ti % 3])
                pi = [0]

                def tok_block(wo_, n, dst_ap, dst_t, eng="dve"):
                    pp = pt[pi[0] % 2]
                    pi[0] += 1
                    P.mm([(pp[:, 0:n], xTt[:, c, :], wB[:, c, wo_:wo_ + n], c == 0, c == 7) for c in range(8)],
                         [xTt, wB], [pp])
                    P.tt(eng, dst_ap, pp[:, 0:n], btok[:, wo_:wo_ + n], ALU.add, [pp, btok], [dst_t])

                for hh in range(2):
                    tok_block(oSV + hh * 512, 512, gv[:, hh * 512:(hh + 1) * 512], gv)
                P.act(gv[:], gv[:], AF.Gelu_apprx_tanh, [gv], [gv])
                P.act(junk2[:], gv[:], AF.Square, [gv], [junk2, sw], accum=sw[:, 0:1])
                P.ts("dve", sw[:, 2:3], sw[:, 0:1], 1.0 / 1024, EPS, ALU.mult, ALU.add, [sw], [sw])
                P.tt("pool", sw[:, 1:2], sw[:, 2:3], mhalf[:], ALU.pow, [sw, mhalf], [sw])
                for hh in range(2):
                    tok_block(oSU + hh * 512, 512, ut[:, hh * 512:(hh + 1) * 512], ut)
                for hh in range(2):
                    tok_block(oG1 + hh * 512, 512, g1[:, hh * 512:(hh + 1) * 512], g1)
                P.stt("dve", vnt[:], gv[:], sw[:, 1:2], gsgu[:], ALU.mult, ALU.mult, [gv, sw, gsgu], [vnt])
                P.copy("act", vb_[:], vnt[:], [vnt], [vb_])
                if sample:
                    P.dma(sguv_o, vnt[:], [vnt], [], "oC", eng="pool")
                P.act(ut[:], ut[:], AF.Gelu_apprx_tanh, [ut], [ut])
                P.act(g1[:], g1[:], AF.Tanh, [g1], [g1], scale=0.5)
                P.ts("dve", g1[:], g1[:], 0.5, 0.5, ALU.mult, ALU.add, [g1], [g1])
                P.tt("dve", ut[:], ut[:], g1[:], ALU.mult, [ut, g1], [ut])

            def backB(ti):
                sample = (ti == NT - 1)
                ma = mat[ti % 3]
                ut = ub2[ti % 2]
                vb_ = vnb2[ti % 2]
                sv_i = 1 if sample else 0
                for g in range(4):
                    pp = pa[g % 2]
                    P.mm([(pp[:, 0:256], wsT[sv_i][:, g, :], vb_[:, g * 256:(g + 1) * 256], True, True)],
                         [wsT[sv_i], vb_], [pp])
                    P.stt("dve", junk[:, g * 256:(g + 1) * 256], pp[:, 0:256], bs[sv_i][:, g:g + 1],
                          ut[:, g * 256:(g + 1) * 256], ALU.add, ALU.mult, [pp, bs[sv_i], ut], [junk])
                P.tt("dve", ma[:], ma[:], junk[:], ALU.add, [ma, junk], [ma])
                P.dma(ma_d[ti * 128:(ti + 1) * 128, :], ma[:], [ma], [ma_d], f"o{ti%2}", eng="pool")
                if p0 is not None:
                    p0.step()

            if p0 is not None:
                p0.load_some()
            if PIPE_B:
                frontB(0)
                for ti in range(NT):
                    if ti + 1 < NT:
                        frontB(ti + 1)
                    backB(ti)
            else:
                for ti in range(NT):
                    frontB(ti)
                    backB(ti)
            if p0 is not None:
                while p0.pending or p0.next_piece < 128:
                    p0.step()
            P.phase_end()

        with ExitStack() as es:
            NCW = 2048
            oXQ, oG2 = 0, 1024
            gm = P.sb(es, "gmC", [128, 8], F32)
            ones8 = P.sb(es, "ones8", [128, 8], F32)
            stg = [P.sb(es, f"stgC{i}", [128, 1024], F32) for i in range(2)]
            wC = P.sb(es, "wC", [128, 8, NCW], BF16)
            wo = P.sb(es, "wo", [128, 8, 1024], BF16)
            bxq = P.sb(es, "bxq", [128, 8], F32)
            btok = P.sb(es, "btokC", [128, 1024], F32)
            gfin = P.sb(es, "gfin", [128, 1024], F32)
            KT = [P.sb(es, f"KT{i}", [128, 8, 256], BF16) for i in range(3)]
            Vb = [P.sb(es, f"Vb{i}", [128, 2, 1024], BF16) for i in range(3)]
            kvs = [P.sb(es, f"kvs{i}", [128, 1024], F32) for i in range(2)]
            kvb = P.sb(es, "kvb", [128, 1024], BF16)
            xT = [P.sb(es, f"xTC{i}", [128, 8, 128], BF16) for i in range(3)]
            xt = [P.sb(es, f"xC{i}", [128, 1024], F32) for i in range(3)]
            mat = [P.sb(es, f"maC{i}", [128, 1024], F32) for i in range(3)]
            kx = ["x0", "x1", "misc0"]
            kxT = ["st0", "st1", "misc1"]
            kma = ["ld2", "ld3", "ws0"]
            junk = P.sb(es, "junkC", [128, 1024], F32)
            sw = P.sb(es, "swC", [128, 16], F32)
            xqTb = [P.sb(es, f"xqT{i}", [128, 8, 128], BF16) for i in range(2)]
            g2b = [P.sb(es, f"g2{i}", [128, 1024], F32) for i in range(2)]
            prh = [P.sb(es, f"prh{i}", [128, 256], BF16) for i in range(4)]
            prTall = P.sb(es, "prTall", [128, 8, 128], BF16)
            sth = [P.sb(es, f"sth{i}", [128, 2], F32) for i in range(4)]
            sth2 = [P.sb(es, f"sth2{i}", [128, 2], F32) for i in range(4)]
            hc = P.sb(es, "hc", [128, 1024], F32)
            hc2 = P.sb(es, "hc2", [128, 1024], F32)
            pr = P.sb(es, "pr", [128, 256], BF16)
            prT = P.sb(es, "prT", [128, 2, 128], BF16)
            mg = P.sb(es, "mg", [128, 1024], BF16)
            mgT = P.sb(es, "mgT", [128, 8, 128], BF16)
            x2 = [P.sb(es, f"x2{i}", [128, 1024], F32) for i in range(2)]
            yt = [P.sb(es, f"yt{i}", [128, 1024], F32) for i in range(2)]
            pT = P.ps(es, "pTC", [128, 8, 128], BF16)
            pf = P.ps(es, "pfC", [128, 4, 128], F32)
            pt = [P.ps(es, f"ptC{i}", [128, 512], F32) for i in range(2)]
            pa = [P.ps(es, f"paC{i}", [128, 512], F32) for i in range(4)]
            po2 = [pt[0], pt[1]]

            P.dma(gm[:], g_mix.rearrange("(c p) -> p c", p=128), [], [gm], "c", allow_slow_non_contiguous=True)
            P.dma(btok[:], b_in[G20:DIN].partition_broadcast(128), [], [btok], "c")
            P.dma(bxq[:], b_in[XQ0:XQ0 + 1024].rearrange("(c p) -> p c", p=128), [], [bxq], "c",
                  allow_slow_non_contiguous=True)
            P.dma(gfin[:], g_final.partition_broadcast(128), [], [gfin], "c")
            P.memset("dve", ones8[:], 1.0, [ones8])
            load_w(es, wC, w_in, [(XQ0, G00), (G20, DIN)], gm, stg)
            load_w(es, wo, w_out, [(0, 1024)], ones8, stg)

            def load_kv(slot, k_dram, v_dram, k_u, v_u):
                for mc in range(2):
                    s_ = kvs[mc % 2]
                    P.dma(s_[:], k_dram[mc * 128:(mc + 1) * 128, :], [k_u] if k_u else [], [s_], f"ld{mc}")
                    P.copy("dve", kvb[:], s_[:], [s_], [kvb])
                    P.tr([(pT[:, c, :], kvb[:, c * 128:(c + 1) * 128], identb[:]) for c in range(8)], [kvb, identb], [pT])
                    P.copy("act", KT[slot][:, :, mc * 128:(mc + 1) * 128], pT[:], [pT], [KT[slot]])
                for mc in range(2):
                    s_ = kvs[mc % 2]
                    P.dma(s_[:], v_dram[mc * 128:(mc + 1) * 128, :], [v_u] if v_u else [], [s_], f"ld{mc}")
                    P.copy("pool", Vb[slot][:, mc, :], s_[:], [s_], [Vb[slot]])

            def frontC(ti):
                sample = (ti == NT - 1)
                x = xt[ti % 3]
                xTt = xT[ti % 3]
                ma = mat[ti % 3]
                g2 = g2b[ti % 2]
                xqT = xqTb[ti % 2]
                if sample:
                    load_kv(0, cmk[0], cmv[0], None, None)
                    load_kv(2, cmk[1], cmv[1], None, None)
                elif ti % NTP == 0:
                    b = ti // NTP
                    load_kv(b, mk_o[b], mv_o[b], mk_u, mv_u)
                P.dma(x[:], xall[ti * 128:(ti + 1) * 128, :], [], [x], kx[ti % 3])
                P.dma(xTt[:], xnT_d[ti].rearrange("p (c t) -> p c t", c=8), [xnT_d], [xTt], kxT[ti % 3])
                P.dma(ma[:], ma_d[ti * 128:(ti + 1) * 128, :], [ma_d], [ma], kma[ti % 3])
                for hh in range(2):
                    pp = pt[hh]
                    P.mm([(pp[:], xTt[:, c, :], wC[:, c, oG2 + hh * 512:oG2 + (hh + 1) * 512], c == 0, c == 7)
                          for c in range(8)], [xTt, wC], [pp])
                    P.tt("dve", g2[:, hh * 512:(hh + 1) * 512], pp[:], btok[:, hh * 512:(hh + 1) * 512], ALU.add,
                         [pp, btok], [g2])
                P.act(g2[:], g2[:], AF.Tanh, [g2], [g2], scale=0.5)
                P.ts("dve", g2[:], g2[:], 0.5, 0.5, ALU.mult, ALU.add, [g2], [g2])
                for j in range(2):
                    for h in range(4):
                        cc = j * 4 + h
                        P.mm([(pf[:, h, :], wC[:, c, oXQ + cc * 128:oXQ + (cc + 1) * 128], xTt[:, c, :], c == 0, c == 7)
                              for c in range(8)], [wC, xTt], [pf])
                    P.tt("dve", xqT[:, j * 4:(j + 1) * 4, :], pf[:],
                         bxq[:, j * 4:(j + 1) * 4].unsqueeze(2).broadcast_to([128, 4, 128]), ALU.add, [pf, bxq], [xqT])

            def attention(dst, slot, g2, xqT):
                for h in range(4):
                    P.mm([(pa[h][:, 0:256], xqT[:, 2 * h + dc, :], KT[slot][:, 2 * h + dc, :], dc == 0, dc == 1)
                          for dc in range(2)], [xqT, KT[slot]], [pa[h]])
                for h in range(4):
                    P.add("dve", lambda e, h=h: e.reduce_max(out=sth[h][:, 0:1], in_=pa[h][:, 0:256], axis=AX.X),
                          [pa[h]], [sth[h]])
                for h in range(4):
                    P.ts("dve", sth[h][:, 1:2], sth[h][:, 0:1], -1.0 / 16, None, ALU.mult, None, [sth[h]], [sth[h]])
                for h in range(4):
                    P.act(prh[h][:], pa[h][:, 0:256], AF.Exp, [pa[h], sth[h]], [prh[h], sth2[h]], bias=sth[h][:, 1:2],
                          scale=1.0 / 16, accum=sth2[h][:, 0:1])
                for h in range(4):
                    P.recip(sth2[h][:, 1:2], sth2[h][:, 0:1], [sth2[h]], [sth2[h]])
                P.tr([(pT[:, 2 * h + mc, :], prh[h][:, mc * 128:(mc + 1) * 128], identb[:])
                      for h in range(4) for mc in range(2)], prh + [identb], [pT])
                P.copy("act", prTall[:], pT[:], [pT], [prTall])
                for h in range(4):
                    P.mm([(pa[h][:, 256:512], prTall[:, 2 * h + mc, :], Vb[slot][:, mc, h * 256:(h + 1) * 256],
                           mc == 0, mc == 1) for mc in range(2)], [prTall, Vb[slot]], [pa[h]])
                for h in range(4):
                    P.stt("dve", dst[:, h * 256:(h + 1) * 256], pa[h][:, 256:512], sth2[h][:, 1:2],
                          g2[:, h * 256:(h + 1) * 256], ALU.mult, ALU.mult, [pa[h], sth2[h], g2], [dst])

            def backC(ti):
                sample = (ti == NT - 1)
                x = xt[ti % 3]
                ma = mat[ti % 3]
                g2 = g2b[ti % 2]
                xqT = xqTb[ti % 2]
                if sample:
                    attention(hc, 0, g2, xqT)
                    attention(hc2, 2, g2, xqT)
                    P.ts("pool", hc[:], hc[:], cst[:, C_MISC, 0:1], None, ALU.mult, None, [hc, cst], [hc])
                    P.stt("dve", hc[:], hc2[:], cst[:, C_MISC, 1:2], hc[:], ALU.mult, ALU.add, [hc2, cst, hc], [hc])
                else:
                    attention(hc, ti // NTP, g2, xqT)
                P.tt("dve", mg[:], ma[:], hc[:], ALU.add, [ma, hc], [mg])
                P.tr([(pT[:, c, :], mg[:, c * 128:(c + 1) * 128], identb[:]) for c in range(8)], [mg, identb], [pT])
                P.copy("act", mgT[:], pT[:], [pT], [mgT])
                xo = x2[ti % 2]
                for hh in range(2):
                    pp = po2[hh]
                    P.mm([(pp[:], mgT[:, c, :], wo[:, c, hh * 512:(hh + 1) * 512], c == 0, c == 7) for c in range(8)],
                         [mgT, wo], [pp])
                    P.tt("dve", xo[:, hh * 512:(hh + 1) * 512], pp[:], x[:, hh * 512:(hh + 1) * 512], ALU.add,
                         [pp, x], [xo])
                P.dma(x2_d[ti * 128:(ti + 1) * 128, :], xo[:], [xo], [x2_d], f"o{ti%2}", eng="pool")
                if not peer:
                    yo = yt[ti % 2]
                    P.act(junk[:], xo[:], AF.Square, [xo], [junk, sw], accum=sw[:, 8:9])
                    P.act(sw[:, 9:10], sw[:, 8:9], AF.Sqrt, [sw], [sw], bias=EPS, scale=1.0 / 1024)
                    P.recip(sw[:, 9:10], sw[:, 9:10], [sw], [sw])
                    P.stt("dve", yo[:], xo[:], sw[:, 9:10], gfin[:], ALU.mult, ALU.mult, [xo, sw, gfin], [yo])
                    P.dma(y_o[ti * 128:(ti + 1) * 128, :], yo[:], [yo], [], f"sc{ti%2}", eng="pool")

            if PIPE_C:
                frontC(0)
                for ti in range(NT):
                    if ti + 1 < NT:
                        frontC(ti + 1)
                    backC(ti)
            else:
                for ti in range(NT):
                    frontC(ti)
                    backC(ti)
            P.phase_end(final=not peer)

        if not peer:
            return nc

        xn2T_d = T(dscr("xn2T_d", [NT, 128, 1024], BF16), "xn2T_d")
        rt_d = T(dscr("rt_d", [NT, 128, 384]), "rt_d")

        with ExitStack() as es:
            ones8 = P.sb(es, "ones8p", [128, 8], F32)
            stg = [P.sb(es, f"stgP{i}", [128, 1024], F32) for i in range(2)]
            wpq = P.sb(es, "wpq", [128, 8, 2048], BF16)
            gffn = P.sb(es, "gffn", [128, 1024], F32)
            ks = P.sb(es, "ks", [128, 2, 128], F32)
            ksT = P.sb(es, "ksT", [128, 2, 128], F32)
            xt = [P.sb(es, f"xP{i}", [128, 1024], F32) for i in range(3)]
            kxp = ["x0", "x1", "misc0"]
            junk = P.sb(es, "junkP", [128, 1024], F32)
            sw = P.sb(es, "swP", [128, 8], F32)
            mhalfP = P.sb(es, "mhalfP", [128, 1], F32)
            P.memset("dve", mhalfP[:], -0.5, [mhalfP])
            xn = P.sb(es, "xnP", [128, 1024], BF16)
            xT = [P.sb(es, f"xTP{i}", [128, 8, 128], BF16) for i in range(2)]
            qT = P.sb(es, "qTP", [128, 16, 128], F32)
            Sb = [P.sb(es, f"S{i}", [128, 16, 128], F32) for i in range(2)]
            xn32 = P.sb(es, "xn32", [128, 1024], F32)
            Sw = P.sb(es, "Sw", [128, 16, 128], F32)
            Sw2 = P.sb(es, "Sw2", [128, 8, 256], F32)
            V = P.sb(es, "V", [128, 16, 16], F32)
            I = P.sb(es, "I", [128, 16, 16], U32)
            Sub = [[T(Sb[k][:, 4 * i:4 * i + 4, :], f"Su{k}{i}") for i in range(4)] for k in range(2)]
            Vu = [T(V[:, j, :], f"Vu{j}") for j in range(16)]
            Iu = [T(I[:, j, :], f"Iu{j}") for j in range(16)]
            Swu = [T(Sw[:, j, :], f"Swu{j}") for j in range(16)]
            If = P.sb(es, "If", [128, 16, 16], F32)
            cand = P.sb(es, "cand", [128, 8, 256], F32)
            scv = P.sb(es, "scv", [128, 8, 16], F32)
            pos = P.sb(es, "pos", [128, 8, 16], U32)
            scu = [T(scv[:, h, :], f"scu{h}") for h in range(8)]
            posu = [T(pos[:, h, :], f"posu{h}") for h in range(8)]
            Sw2u = [T(Sw2[:, h, :], f"Sw2u{h}") for h in range(8)]
            pa_ = P.sb(es, "pa_", [128, 8, 16], U32)
            pb_ = P.sb(es, "pb_", [128, 8, 16], U32)
            af = P.sb(es, "af", [128, 8, 16], F32)
            bf = P.sb(es, "bf", [128, 8, 16], F32)
            oh = P.sb(es, "oh", [128, 8, 16, 16], F32)
            oh2 = P.sb(es, "oh2", [128, 8, 16, 16], F32)
            ex = P.sb(es, "ex", [128, 8, 16], F32)
            sm8 = P.sb(es, "sm8", [128, 8], F32)
            res = P.sb(es, "res", [128, 3, 128], F32)
            rt = [P.sb(es, f"rt{i}", [128, 3, 128], F32) for i in range(2)]
            pT = P.ps(es, "pTP", [128, 8, 128], BF16)
            pq = [P.ps(es, f"pq{i}", [128, 4, 128], F32) for i in range(4)]
            ptr = P.ps(es, "ptr", [128, 3, 128], F32)

            P.dma(gffn[:], g_ffn.partition_broadcast(128), [], [gffn], "c")
            P.dma(ks[:, 0, :], k_sub1, [], [ks], "c")
            P.dma(ks[:, 1, :], k_sub2, [], [ks], "c")
            P.memset("dve", ones8[:], 1.0, [ones8])
            load_w(es, wpq, w_pq, [(0, 2048)], ones8, stg)
            P.tr([(ptr[:, i, :], ks[:, i, :], identf) for i in range(2)], [ks, cst], [ptr])
            P.copy("dve", ksT[:], ptr[:, 0:2, :], [ptr], [ksT])
            NEG = -1.0e30

            def top16_rows(rows):
                for (src, su, wu, vu, iu) in rows:
                    P.add("dve", lambda e, src=src, vu=vu: e.max(out=vu[:, 0:8], in_=src), [su], [vu])
                for (src, su, wu, vu, iu) in rows:
                    P.add("dve", lambda e, src=src, vu=vu, iu=iu: e.max_index(out=iu[:, 0:8], in_max=vu[:, 0:8],
                                                                            in_values=src), [su, vu], [iu])
                for (src, su, wu, vu, iu) in rows:
                    P.add("dve", lambda e, src=src, vu=vu, wu=wu: e.match_replace(
                        out=wu[:], in_to_replace=vu[:, 0:8], in_values=src, imm_value=NEG), [su, vu], [wu])
                for (src, su, wu, vu, iu) in rows:
                    P.add("dve", lambda e, vu=vu, wu=wu: e.max(out=vu[:, 8:16], in_=wu[:]), [wu], [vu])
                for (src, su, wu, vu, iu) in rows:
                    P.add("dve", lambda e, vu=vu, wu=wu, iu=iu: e.max_index(out=iu[:, 8:16], in_max=vu[:, 8:16],
                                                                           in_values=wu[:]), [wu, vu], [iu])

            def front(ti):
                x = xt[ti % 3]
                xTt = xT[ti % 2]
                S = Sb[ti % 2]
                Su = Sub[ti % 2]
                P.dma(x[:], x2_d[ti * 128:(ti + 1) * 128, :], [x2_d], [x], kxp[ti % 3])
                P.act(junk[:], x[:], AF.Square, [x], [junk, sw], accum=sw[:, 0:1])
                P.ts("dve", sw[:, 2:3], sw[:, 0:1], 1.0 / 1024, EPS, ALU.mult, ALU.add, [sw], [sw])
                P.tt("pool", sw[:, 1:2], sw[:, 2:3], mhalfP[:], ALU.pow, [sw, mhalfP], [sw])
                P.stt("dve", xn[:], x[:], sw[:, 1:2], gffn[:], ALU.mult, ALU.mult, [x, sw, gffn], [xn])
                P.tr([(pT[:, c, :], xn[:, c * 128:(c + 1) * 128], identb[:]) for c in range(8)], [xn, identb], [pT])
                P.copy("act", xTt[:], pT[:], [pT], [xTt])
                P.dma(xn2T_d[ti].rearrange("p (c t) -> p c t", c=8), xTt[:], [xTt], [xn2T_d], f"pst{ti%2}", eng="pool")
                for bnk in range(4):
                    for jj in range(4):
                        j = bnk * 4 + jj
                        P.mm([(pq[bnk][:, jj, :], wpq[:, c, j * 128:(j + 1) * 128], xTt[:, c, :], c == 0, c == 7)
                              for c in range(8)], [wpq, xTt], [pq[bnk]])
                    P.copy("act", qT[:, bnk * 4:(bnk + 1) * 4, :], pq[bnk][:], [pq[bnk]], [qT])
                for bnk in range(4):
                    P.mm([(pq[bnk][:, jj, :], qT[:, bnk * 4 + jj, :], ksT[:, (bnk * 4 + jj) % 2, :], True, True)
                          for jj in range(4)], [qT, ksT], [pq[bnk]])
                    P.copy("act", S[:, bnk * 4:(bnk + 1) * 4, :], pq[bnk][:], [pq[bnk]], [Su[bnk]])

            def chain(ti):
                S = Sb[ti % 2]
                Su = Sub[ti % 2]
                rtt = rt[ti % 2]
                top16_rows([(S[:, j, :], Su[j // 4], Swu[j], Vu[j], Iu[j]) for j in range(16)])
                Vv = V[:].rearrange("p (h two) r -> p h two r", two=2)
                Ifv = If[:].rearrange("p (h two) r -> p h two r", two=2)
                P.tt("pool", cand[:].rearrange("p h (a b) -> p h a b", a=16),
                     Vv[:, :, 0, :].unsqueeze(3).broadcast_to([128, 8, 16, 16]),
                     Vv[:, :, 1, :].unsqueeze(2).broadcast_to([128, 8, 16, 16]), ALU.add, Vu, [cand])
                P.copy("dve", If[:], I[:], Iu, [If])
                top16_rows([(cand[:, h, :], cand, Sw2u[h], scu[h], posu[h]) for h in range(8)])
                iota16 = cst[:, C_IOTA, 0:16].unsqueeze(1).unsqueeze(1).broadcast_to([128, 8, 16, 16])
                P.tt("dve", ex[:], scv[:], scv[:, :, 0:1].broadcast_to([128, 8, 16]), ALU.subtract, scu, [ex])
                P.act(ex[:], ex[:], AF.Exp, [ex], [ex])
                P.add("dve", lambda e: e.tensor_single_scalar(out=pa_[:], in_=pos[:], scalar=4,
                                                              op=ALU.logical_shift_right), posu, [pa_])
                P.add("dve", lambda e: e.tensor_single_scalar(out=pb_[:], in_=pos[:], scalar=15, op=ALU.bitwise_and),
                      posu, [pb_])
                P.copy("dve", af[:], pa_[:], [pa_], [af])
                P.copy("dve", bf[:], pb_[:], [pb_], [bf])
                P.tt("dve", oh[:], iota16, af[:].unsqueeze(3).broadcast_to([128, 8, 16, 16]), ALU.is_equal,
                     [cst, af], [oh])
                P.tt("dve", oh2[:], iota16, bf[:].unsqueeze(3).broadcast_to([128, 8, 16, 16]), ALU.is_equal,
                     [cst, bf], [oh2])
                P.tt("dve", oh[:], oh[:], Ifv[:, :, 0, :].unsqueeze(2).broadcast_to([128, 8, 16, 16]), ALU.mult,
                     [oh, If], [oh])
                P.tt("dve", oh2[:], oh2[:], Ifv[:, :, 1, :].unsqueeze(2).broadcast_to([128, 8, 16, 16]), ALU.mult,
                     [oh2, If], [oh2])
                P.add("dve", lambda e: e.reduce_sum(out=sm8[:], in_=ex[:], axis=AX.X), [ex], [sm8])
                P.recip(sm8[:], sm8[:], [sm8], [sm8])
                P.tt("dve", res[:, 2, :].rearrange("p (h r) -> p h r", h=8), ex[:],
                     sm8[:].unsqueeze(2).broadcast_to([128, 8, 16]), ALU.mult, [ex, sm8], [res])
                P.add("dve", lambda e: e.reduce_sum(out=res[:, 0, :].rearrange("p (h r) -> p h r", h=8),
                                                    in_=oh[:], axis=AX.X), [oh], [res])
                P.add("dve", lambda e: e.reduce_sum(out=res[:, 1, :].rearrange("p (h r) -> p h r", h=8),
                                                    in_=oh2[:], axis=AX.X), [oh2], [res])
                P.tr([(ptr[:, w, :], res[:, w, :], identf) for w in range(3)], [res, cst], [ptr])
                P.copy("act", rtt[:], ptr[:], [ptr], [rtt])
                P.dma(rt_d[ti].rearrange("p (w t) -> p w t", w=3), rtt[:], [rtt], [rt_d], f"o{ti%2}", eng="pool")

            front(0)
            for ti in range(NT):
                if ti + 1 < NT:
                    front(ti + 1)
                chain(ti)
            P.phase_end()

        with ExitStack() as es:
            GT = 3
            G = GT * 128
            TB = 8
            gfin = P.sb(es, "gfinP", [128, 1024], F32)
            WT = P.sb(es, "WT", [128, G, 128], BF16)
            xg = P.sb(es, "xg", [128, 8, G], BF16)
            x2g = [P.sb(es, f"x2g{i}", [128, 1024], F32) for i in range(GT)]
            rtg = [P.sb(es, f"rtg{i}", [128, 3, 128], F32) for i in range(GT)]
            Aoh = [P.sb(es, f"Aoh{i}", [128, TB, 128], BF16) for i in range(2)]
            Boh = [P.sb(es, f"Boh{i}", [128, TB, 128], BF16) for i in range(2)]
            iotab = P.sb(es, "iotab", [128, 128], BF16)
            ub = [P.sb(es, f"ub{i}", [128, 8, 512], BF16) for i in range(3)]
            vb = [P.sb(es, f"vbb{i}", [128, 4, 1024], BF16) for i in range(3)]
            hg = [P.sb(es, f"hg{i}", [128, G], F32) for i in range(2)]
            GA = [P.sb(es, f"GA{i}", [128, G], BF16) for i in range(2)]
            x3 = P.sb(es, "x3", [128, 1024], F32)
            sw = P.sb(es, "swQ", [128, 8], F32)
            yt = [P.sb(es, f"ytQ{i}", [128, 1024], F32) for i in range(2)]
            po = [[P.ps(es, f"po{t}{h}", [128, 512], F32) for h in range(2)] for t in range(GT)]
            ph = [P.ps(es, f"ph{i}", [128, 512], F32) for i in range(2)]
            uk = ["ld0", "ld1", "ld2"]
            vk = ["ws0", "ws1", "ld3"]
            P.dma(gfin[:], g_final.partition_broadcast(128), [], [gfin], "c")
            P.copy("dve", iotab[:], cst[:, C_IOTA, :], [cst], [iotab])
            yi = 0
            blk = 0
            for g0 in range(0, NT, GT):
                tiles = list(range(g0, min(NT, g0 + GT)))
                ng = len(tiles) * 128
                for tt, ti in enumerate(tiles):
                    P.dma(rtg[tt][:], rt_d[ti].rearrange("p (w t) -> p w t", w=3), [rt_d], [rtg[tt]], "misc0")
                for tt, ti in enumerate(tiles):
                    P.dma(x2g[tt][:], x2_d[ti * 128:(ti + 1) * 128, :], [x2_d], [x2g[tt]], "x0")
                    P.dma(xg[:, :, tt * 128:(tt + 1) * 128], xn2T_d[ti].rearrange("p (c t) -> p c t", c=8), [xn2T_d],
                          [xg], "st0")
                wi = 0
                for tt, ti in enumerate(tiles):
                    for sbk in range(128 // TB):
                        A = Aoh[sbk % 2]
                        B = Boh[sbk % 2]
                        t0 = sbk * TB
                        for k in range(TB):
                            t = t0 + k
                            P.ts("dve", A[:, k, :], iotab[:], rtg[tt][:, 0, t:t + 1], rtg[tt][:, 2, t:t + 1],
                                 ALU.is_equal, ALU.mult, [iotab, rtg[tt]], [A])
                            P.ts("dve", B[:, k, :], iotab[:], rtg[tt][:, 1, t:t + 1], None, ALU.is_equal, None,
                                 [iotab, rtg[tt]], [B])
                        for q4 in range(TB // 4):
                            pwb = ph[wi % 2]
                            wi += 1
                            P.mm([(pwb[:, k * 128:(k + 1) * 128], B[:, q4 * 4 + k, :], A[:, q4 * 4 + k, :], True, True)
                                  for k in range(4)], [A, B], [pwb])
                            tg = tt * 128 + t0 + q4 * 4
                            P.copy("act", WT[:, tg:tg + 4, :].rearrange("p t i -> p (t i)"), pwb[:], [pwb], [WT])
                bufs = {}

                def load_block(cb4):
                    nonlocal blk
                    ui = blk % 3
                    blk += 1
                    P.dma(ub[ui][:], u_bf[cb4], [u_bf], [ub[ui]], uk[ui])
                    P.dma(vb[ui][:], v_bf[cb4], [v_bf], [vb[ui]], vk[ui])
                    bufs[cb4] = ui

                def emit_H(c):
                    ui = bufs[c // 4]
                    cl = c % 4
                    phb = ph[c % 2]
                    P.mm([(phb[:, 0:ng], ub[ui][:, dc, cl * 128:(cl + 1) * 128], xg[:, dc, 0:ng], dc == 0, dc == 7)
                          for dc in range(8)], [ub[ui], xg], [phb])

                load_block(0)
                load_block(1)
                emit_H(0)
                for c in range(128):
                    if c % 4 == 0 and c // 4 + 2 < 32:
                        load_block(c // 4 + 2)
                    if c + 1 < 128:
                        emit_H(c + 1)
                    ui = bufs[c // 4]
                    cl = c % 4
                    phb = ph[c % 2]
                    hgb = hg[c % 2]
                    gab = GA[c % 2]
                    P.act(hgb[:, 0:ng], phb[:, 0:ng], AF.Gelu_apprx_tanh, [phb], [hgb])
                    P.tt("pool" if c % 2 else "dve", gab[:, 0:ng], hgb[:, 0:ng], WT[:, 0:ng, c], ALU.mult,
                         [hgb, WT], [gab])
                    lst = []
                    for tt in range(len(tiles)):
                        for hh in range(2):
                            lst.append((po[tt][hh][:], gab[:, tt * 128:(tt + 1) * 128],
                                        vb[ui][:, cl, hh * 512:(hh + 1) * 512], c == 0, c == 127))
                    P.mm(lst, [gab, vb[ui]], [po[tt][hh] for tt in range(len(tiles)) for hh in range(2)])
                for tt, ti in enumerate(tiles):
                    yo = yt[yi % 2]
                    for hh in range(2):
                        P.tt("dve", x3[:, hh * 512:(hh + 1) * 512], po[tt][hh][:], x2g[tt][:, hh * 512:(hh + 1) * 512],
                             ALU.add, [po[tt][hh], x2g[tt]], [x3])
                    P.act(yo[:], x3[:], AF.Square, [x3], [yo, sw], accum=sw[:, 0:1])
                    P.act(sw[:, 1:2], sw[:, 0:1], AF.Sqrt, [sw], [sw], bias=EPS, scale=1.0 / 1024)
                    P.recip(sw[:, 1:2], sw[:, 1:2], [sw], [sw])
                    P.stt("dve", yo[:], x3[:], sw[:, 1:2], gfin[:], ALU.mult, ALU.mult, [x3, sw, gfin], [yo])
                    P.dma(y_o[ti * 128:(ti + 1) * 128, :], yo[:], [yo], [], f"sc{yi%2}", eng="pool")
                    yi += 1
            P.phase_end(final=True)
    return nc


def shard_inputs(inp, NTP=16):
    f = lambda a: np.ascontiguousarray(np.asarray(a, dtype=np.float32))
    xp, xs = f(inp["x_prompt"]), f(inp["x_sample"])
    consts = make_consts()
    u_expT = np.ascontiguousarray(f(inp["u_exp"])[0].T)
    shared = {
        "g_mix": f(inp["g_mix"])[0], "w_in": f(inp["w_in"])[0], "b_in": f(inp["b_in"])[0],
        "g_mlh": f(inp["g_mlh"])[0], "g_sgu": f(inp["g_sgu"])[0], "w_s": f(inp["w_s"])[0], "b_s": f(inp["b_s"])[0],
        "g_mem": f(inp["g_mem"])[0], "w_mk": f(inp["w_mk"])[0], "w_mv": f(inp["w_mv"])[0], "w_out": f(inp["w_out"])[0],
        "g_ffn": f(inp["g_ffn"])[0], "w_pq": f(inp["w_pq"])[0], "k_sub1": f(inp["k_sub1"])[0],
        "k_sub2": f(inp["k_sub2"])[0], "u_expT": u_expT, "v_exp": f(inp["v_exp"])[0], "g_final": f(inp["g_final"]),
        "consts": consts,
    }
    maps = []
    for c in range(8):
        b0, b1 = 2 * c, 2 * c + 2
        m = dict(shared)
        m["xall"] = np.ascontiguousarray(np.concatenate(
            [xp[b0:b1].reshape(-1, 1024), xs[b0:b1].reshape(-1, 1024)], axis=0))
        m["mem"] = np.ascontiguousarray(f(inp["mem_prompt"])[b0:b1].reshape(512, 1024))
        m["cmk"] = np.ascontiguousarray(f(inp["cache_mem_k"])[0, b0:b1].reshape(2, 256, 1024))
        m["cmv"] = np.ascontiguousarray(f(inp["cache_mem_v"])[0, b0:b1].reshape(2, 256, 1024))
        m["sC"] = np.ascontiguousarray(f(inp["state_mlstm_C"])[0, b0:b1])
        m["sn"] = np.ascontiguousarray(f(inp["state_mlstm_n"])[0, b0:b1])
        m["sm"] = np.ascontiguousarray(f(inp["state_mlstm_m"])[0, b0:b1])
        maps.append(m)
    return maps


def gather_outputs(results, NTP=16):
    S = NTP * 128
    cat = lambda k: np.concatenate([r[k] for r in results], axis=0)
    y = [r["y"] for r in results]
    y_prompt = np.concatenate([a[:2 * S].reshape(2, S, 1024) for a in y], axis=0)
    y_sample = np.concatenate([a[2 * S:].reshape(2, 64, 1024) for a in y], axis=0)
    return (y_prompt, y_sample, cat("Cp")[None], cat("np")[None], cat("mp")[None],
            cat("mk").reshape(16, 256, 4, 256)[None], cat("mv").reshape(16, 256, 4, 256)[None],
            cat("Cs")[None], cat("ns")[None], cat("ms")[None],
            np.concatenate([r["sguv"].reshape(2, 64, 1024) for r in results], axis=0)[None])


_NC_CACHE = {}


def kernel(**inputs):
    NTP = inputs["x_prompt"].shape[1] // 128
    peer = inputs.pop("_peer", True) if "_peer" in inputs else True
    key = (NTP, peer)
    if key not in _NC_CACHE:
        _NC_CACHE[key] = build(NTP, peer)
    nc = _NC_CACHE[key]
    maps = shard_inputs(inputs, NTP)
    res = run_bass_kernel_spmd(nc, maps, core_ids=list(range(8)))
    outs = gather_outputs(res.results, NTP)
    return tuple(np.ascontiguousarray(o, dtype=np.float32) for o in outs)
```
